# Optimizing a Trainium2 kernel written in Bass

```python
import jax, jax.numpy as jnp
from jax import lax
import numpy as np

D_MODEL = 1024
BATCH = 8
SEQ = 4096
DEPTH = 4

HEAD_DIM = 64
MEM_DIM = D_MODEL // 4
MEM_HEADS = MEM_DIM // HEAD_DIM
MEM_LEN = 256
FOX_DIM = D_MODEL - MEM_DIM
FOX_HEADS = FOX_DIM // HEAD_DIM
CONV_DIM = D_MODEL - MEM_DIM
CONV_WIDTH = 31
Q_BLOCK = 128
D_FF = ((8 * D_MODEL // 3 + 127) // 128) * 128
N_EXPERTS = 8
TOP_K = 2
EXPERT_FF = 7 * D_MODEL // 2
EXPERT_BLOCK = 128
N_FOX = (DEPTH + 1) // 2
N_CONV = DEPTH // 2
EPS = 1e-6
NEG_INF = -1e30

kernel_name = "fox_conformer_memory_moe_hybrid"


def rms_norm(x, g):
    xf = x.astype(jnp.float32)
    y = xf * lax.rsqrt(jnp.mean(xf * xf, axis=-1, keepdims=True) + EPS)
    return (y * g.astype(jnp.float32)).astype(x.dtype)


def layer_norm(x, g, b):
    xf = x.astype(jnp.float32)
    mu = jnp.mean(xf, axis=-1, keepdims=True)
    var = jnp.mean(jnp.square(xf - mu), axis=-1, keepdims=True)
    y = (xf - mu) * lax.rsqrt(var + EPS)
    return (y * g.astype(jnp.float32) + b.astype(jnp.float32)).astype(x.dtype)


def forget_attention(q, k, v, log_f):
    B, S, H, hd = q.shape
    nb = S // Q_BLOCK
    c = jnp.cumsum(log_f, axis=1).transpose(0, 2, 1)
    q_blocks = q.reshape(B, nb, Q_BLOCK, H, hd).transpose(1, 0, 2, 3, 4)
    c_blocks = c.reshape(B, H, nb, Q_BLOCK).transpose(2, 0, 1, 3)
    k_pos = jnp.arange(S)
    scale = hd ** -0.5

    def one_block(args):
        q_b, c_b, blk = args
        s = jnp.einsum('bqhd,bkhd->bhqk', q_b, k, preferred_element_type=jnp.float32) * scale
        s = s + (c_b[..., :, None] - c[:, :, None, :])
        q_pos = blk * Q_BLOCK + jnp.arange(Q_BLOCK)
        s = jnp.where(k_pos[None, :] <= q_pos[:, None], s, NEG_INF)
        p = jax.nn.softmax(s, axis=-1).astype(v.dtype)
        return jnp.einsum('bhqk,bkhd->bqhd', p, v)

    o = lax.map(one_block, (q_blocks, c_blocks, jnp.arange(nb)))
    return o.transpose(1, 0, 2, 3, 4).reshape(B, S, H * hd)


def memory_attention(xq, mk, mv, g_q, g_k):
    B, S, _ = xq.shape
    M = mk.shape[1]
    q = rms_norm(xq.reshape(B, S, MEM_HEADS, HEAD_DIM), g_q)
    k = rms_norm(mk.reshape(B, M, MEM_HEADS, HEAD_DIM), g_k)
    v = mv.reshape(B, M, MEM_HEADS, HEAD_DIM)
    s = jnp.einsum('bshd,bmhd->bhsm', q, k, preferred_element_type=jnp.float32) * (HEAD_DIM ** -0.5)
    p = jax.nn.softmax(s, axis=-1).astype(v.dtype)
    return jnp.einsum('bhsm,bmhd->bshd', p, v).reshape(B, S, MEM_DIM)


def conformer_conv(glu_in, dw, dw_b, ln_g, ln_b):
    a, gate = jnp.split(glu_in, 2, axis=-1)
    u = a * jax.nn.sigmoid(gate)
    u = lax.conv_general_dilated(
        u, dw[:, None, :].astype(u.dtype), window_strides=(1,),
        padding=[(CONV_WIDTH - 1, 0)], dimension_numbers=('NWC', 'WIO', 'NWC'),
        feature_group_count=CONV_DIM) + dw_b.astype(u.dtype)
    return jax.nn.silu(layer_norm(u, ln_g, ln_b))


def swiglu(h, w_gate, w_up, w_down):
    return (jax.nn.silu(h @ w_gate) * (h @ w_up)) @ w_down


def moe_swiglu(h, w_router, w_gate, w_up, w_down):
    B, S, D = h.shape
    xt = h.reshape(-1, D)
    T = xt.shape[0]
    A = T * TOP_K
    logits = (xt @ w_router).astype(jnp.float32)
    top_logit, top_idx = lax.top_k(logits, TOP_K)
    gates = jax.nn.softmax(top_logit, axis=-1)
    flat_e = top_idx.reshape(-1)
    order = jnp.argsort(flat_e)
    sorted_e = flat_e[order]
    sizes = jnp.bincount(flat_e, length=N_EXPERTS).astype(jnp.int32)
    padded = ((sizes + EXPERT_BLOCK - 1) // EXPERT_BLOCK) * EXPERT_BLOCK
    pad_end = jnp.cumsum(padded)
    pad_start = pad_end - padded
    start = jnp.cumsum(sizes) - sizes
    dest = pad_start[sorted_e] + (jnp.arange(A, dtype=jnp.int32) - start[sorted_e])
    n_blocks = -(-A // EXPERT_BLOCK) + N_EXPERTS
    rows = n_blocks * EXPERT_BLOCK
    tok_buf = jnp.full((rows,), T, jnp.int32).at[dest].set((order // TOP_K).astype(jnp.int32))
    gate_buf = jnp.zeros((rows,), jnp.float32).at[dest].set(gates.reshape(-1)[order])
    block_start = jnp.arange(n_blocks, dtype=jnp.int32) * EXPERT_BLOCK
    block_expert = jnp.minimum(jnp.searchsorted(pad_end, block_start, side='right'), N_EXPERTS - 1)
    xt_pad = jnp.concatenate([xt, jnp.zeros((1, D), xt.dtype)], axis=0)
    xs = xt_pad[tok_buf].reshape(n_blocks, EXPERT_BLOCK, D)

    def expert_block(args):
        xb, e = args
        return swiglu(xb, w_gate[e], w_up[e], w_down[e])

    ys = lax.map(expert_block, (xs, block_expert)).reshape(rows, D)
    ys = ys * gate_buf[:, None].astype(ys.dtype)
    out = jnp.zeros((T + 1, D), ys.dtype).at[tok_buf].add(ys)[:T]
    return out.reshape(B, S, D)


def setup_inputs(seed: int = 0) -> dict:
    key = jax.random.key(seed)
    ks = iter(jax.random.split(key, 40))

    def nrm(shape, scale):
        return jax.random.normal(next(ks), shape, jnp.float32) * scale

    D = D_MODEL
    fox_in = 3 * FOX_DIM + FOX_HEADS + MEM_DIM
    conv_in = 2 * CONV_DIM + MEM_DIM
    return {
        "x": nrm((BATCH, SEQ, D), 1.0),
        "mem": nrm((BATCH, MEM_LEN, D), 1.0),
        "norm_mix": 1.0 + nrm((DEPTH, D), 0.02),
        "norm_mem": 1.0 + nrm((DEPTH, D), 0.02),
        "norm_ffn": 1.0 + nrm((DEPTH, D), 0.02),
        "w_mem_kv": nrm((DEPTH, D, 2 * MEM_DIM), D ** -0.5),
        "g_mq": 1.0 + nrm((DEPTH, HEAD_DIM), 0.02),
        "g_mk": 1.0 + nrm((DEPTH, HEAD_DIM), 0.02),
        "fox_w_in": nrm((N_FOX, D, fox_in), D ** -0.5),
        "fox_b_f": 3.0 + nrm((N_FOX, FOX_HEADS), 0.5),
        "fox_g_q": 1.0 + nrm((N_FOX, HEAD_DIM), 0.02),
        "fox_g_k": 1.0 + nrm((N_FOX, HEAD_DIM), 0.02),
        "fox_w_out": nrm((N_FOX, FOX_DIM + MEM_DIM, D), 0.5 * D ** -0.5),
        "conv_w_in": nrm((N_CONV, D, conv_in), D ** -0.5),
        "conv_b_in": nrm((N_CONV, 2 * CONV_DIM), 0.02),
        "conv_dw": nrm((N_CONV, CONV_WIDTH, CONV_DIM), CONV_WIDTH ** -0.5),
        "conv_dw_b": nrm((N_CONV, CONV_DIM), 0.02),
        "conv_ln_g": 1.0 + nrm((N_CONV, CONV_DIM), 0.02),
        "conv_ln_b": nrm((N_CONV, CONV_DIM), 0.02),
        "conv_w_out": nrm((N_CONV, CONV_DIM + MEM_DIM, D), 0.5 * D ** -0.5),
        "ffn_w_gate": nrm((N_FOX, D, D_FF), D ** -0.5),
        "ffn_w_up": nrm((N_FOX, D, D_FF), D ** -0.5),
        "ffn_w_down": nrm((N_FOX, D_FF, D), 0.5 * D_FF ** -0.5),
        "moe_router": nrm((N_CONV, D, N_EXPERTS), D ** -0.5),
        "moe_w_gate": nrm((N_CONV, N_EXPERTS, D, EXPERT_FF), D ** -0.5),
        "moe_w_up": nrm((N_CONV, N_EXPERTS, D, EXPERT_FF), D ** -0.5),
        "moe_w_down": nrm((N_CONV, N_EXPERTS, EXPERT_FF, D), 0.5 * EXPERT_FF ** -0.5),
    }


def reference(x, mem, norm_mix, norm_mem, norm_ffn, w_mem_kv, g_mq, g_mk,
              fox_w_in, fox_b_f, fox_g_q, fox_g_k, fox_w_out,
              conv_w_in, conv_b_in, conv_dw, conv_dw_b, conv_ln_g, conv_ln_b, conv_w_out,
              ffn_w_gate, ffn_w_up, ffn_w_down,
              moe_router, moe_w_gate, moe_w_up, moe_w_down):
    B, S, D = x.shape
    h = x
    for i in range(DEPTH):
        j = i // 2
        u = rms_norm(h, norm_mix[i])
        m = rms_norm(mem, norm_mem[i])
        mk, mv = jnp.split(m @ w_mem_kv[i], 2, axis=-1)
        if i % 2 == 0:
            proj = u @ fox_w_in[j]
            q, k, v, f_logit, mq = jnp.split(
                proj, [FOX_DIM, 2 * FOX_DIM, 3 * FOX_DIM, 3 * FOX_DIM + FOX_HEADS], axis=-1)
            q = rms_norm(q.reshape(B, S, FOX_HEADS, HEAD_DIM), fox_g_q[j])
            k = rms_norm(k.reshape(B, S, FOX_HEADS, HEAD_DIM), fox_g_k[j])
            v = v.reshape(B, S, FOX_HEADS, HEAD_DIM)
            log_f = jax.nn.log_sigmoid((f_logit + fox_b_f[j]).astype(jnp.float32))
            mix = forget_attention(q, k, v, log_f)
            w_out = fox_w_out[j]
        else:
            proj = u @ conv_w_in[j]
            glu_in, mq = jnp.split(proj, [2 * CONV_DIM], axis=-1)
            mix = conformer_conv(glu_in + conv_b_in[j], conv_dw[j], conv_dw_b[j],
                                 conv_ln_g[j], conv_ln_b[j])
            w_out = conv_w_out[j]
        mem_out = memory_attention(mq, mk, mv, g_mq[i], g_mk[i])
        h = h + jnp.concatenate([mix, mem_out], axis=-1) @ w_out
        z = rms_norm(h, norm_ffn[i])
        if i % 2 == 0:
            h = h + swiglu(z, ffn_w_gate[j], ffn_w_up[j], ffn_w_down[j])
        else:
            h = h + moe_swiglu(z, moe_router[j], moe_w_gate[j], moe_w_up[j], moe_w_down[j])
    return h
```

```python
from contextlib import ExitStack
import numpy as np
import concourse.bass as bass
import concourse.mybir as mybir
from concourse.bass_utils import run_bass_kernel_spmd

F32 = mybir.dt.float32
BF16 = mybir.dt.bfloat16
AF = mybir.ActivationFunctionType
ALU = mybir.AluOpType
AX = mybir.AxisListType

ENGS = ["pe", "act", "dve", "pool", "sp"]
BLOCKNAME = {"pe": "tensor", "act": "scalar", "dve": "vector", "pool": "gpsimd", "sp": "sync"}

S_TOK = 4096
D = 1024
NT = S_TOK // 128
NTB = S_TOK // 512
EPS = 1e-6
DFF = 2816
EFF = 3584
NEXP = 8


class Reg:
    __slots__ = ("w", "rs")

    def __init__(self):
        self.w = None
        self.rs = {}


def regs(n):
    return [Reg() for _ in range(n)]


class Sched:
    def __init__(self, nc):
        self.nc = nc
        self.ops = {e: [] for e in ENGS}
        self.done = {e: 0 for e in ENGS}
        self.cnt = {e: 0 for e in ENGS}
        self.waited = {e: {} for e in ENGS}
        self.dma_cnt = []
        self.dma_names = {}
        self.pstack = ExitStack()
        self.esem = {e: self.pstack.enter_context(nc.semaphore(f"es_{e}")) for e in ENGS}
        self.dsems = []
        self.n = 0

    def dsem(self, name):
        if name not in self.dma_names:
            self.dma_names[name] = len(self.dma_cnt)
            self.dma_cnt.append(0)
            self.dsems.append(self.pstack.enter_context(self.nc.semaphore(f"ds_{len(self.dsems)}")))
        return self.dma_names[name]

    def _need(self, eng, ticket, waits, raw):
        if ticket is None:
            return
        kind, key, val = ticket
        if kind == "E" and key == eng and (eng == "pe" or not raw):
            return
        k = (kind, key)
        if self.waited[eng].get(k, -1) >= val:
            return
        self.waited[eng][k] = val
        waits.append(ticket)
        if kind == "E":
            o = self.ops[key][val]
            assert not o.get("emitted") or o["sig"], "dependency on already-emitted unsignalled op"
            o["sig"] = True

    def _deps(self, eng, reads, writes):
        waits = []
        for r in reads:
            self._need(eng, r.w, waits, True)
        for r in writes:
            self._need(eng, r.w, waits, False)
            for (kind, key), val in r.rs.items():
                self._need(eng, (kind, key, val), waits, False)
        return waits

    def _commit(self, t, reads, writes):
        k = (t[0], t[1])
        for r in reads:
            if r.rs.get(k, -1) < t[2]:
                r.rs[k] = t[2]
        for r in writes:
            r.w = t
            r.rs = {}

    def op(self, eng, fn, reads=(), writes=()):
        waits = self._deps(eng, reads, writes)
        idx = len(self.ops[eng])
        self.ops[eng].append(dict(fn=fn, waits=waits, sig=False, dma=None))
        t = ("E", eng, idx)
        self._commit(t, reads, writes)
        return t

    def dma(self, q, out_ap, in_ap, reads=(), writes=(), sem="d"):
        waits = self._deps(q, reads, writes)
        sid = self.dsem(sem)
        if self.dma_cnt[sid] > 0:
            self._need(q, ("D", sid, self.dma_cnt[sid]), waits, True)
        self.dma_cnt[sid] += 16
        t = ("D", sid, self.dma_cnt[sid])
        self.ops[q].append(dict(fn=lambda e: e.dma_start(out=out_ap, in_=in_ap), waits=waits, sig=False, dma=sid))
        self._commit(t, reads, writes)
        return t

    def wait_only(self, eng, tickets):
        waits = []
        for t in tickets:
            self._need(eng, t, waits, True)
        self.ops[eng].append(dict(fn=None, waits=waits, sig=False, dma=None))

    def barrier(self):
        tickets = []
        for e in ENGS:
            for idx in range(len(self.ops[e]) - 1, self.done[e] - 1, -1):
                o = self.ops[e][idx]
                if o["fn"] is not None and o["dma"] is None:
                    tickets.append(("E", e, idx))
                    break
        for sid, c in enumerate(self.dma_cnt):
            if c > 0:
                tickets.append(("D", sid, c))
        for e in ENGS:
            self.wait_only(e, tickets)

    def flush(self):
        self.barrier()
        nc = self.nc
        ops = self.ops
        for e in ENGS:
            c = self.cnt[e]
            for o in ops[e][self.done[e]:]:
                if o["sig"]:
                    c += 1
                o["cnt"] = c
                o["emitted"] = True
            self.cnt[e] = c
        esem, dsems = self.esem, self.dsems

        def body(eng, e):
            for o in ops[e][self.done[e]:]:
                for (kind, key, val) in o["waits"]:
                    if kind == "E":
                        eng.wait_ge(esem[key], ops[key][val]["cnt"])
                    else:
                        eng.wait_ge(dsems[key], val)
                if o["fn"] is None:
                    continue
                ins = o["fn"](eng)
                if o["dma"] is not None:
                    ins.then_inc(dsems[o["dma"]], 16)
                elif o["sig"]:
                    ins.then_inc(esem[e], 1)

        with nc.Block() as block:
            for e in ENGS:
                if len(ops[e]) > self.done[e]:
                    getattr(block, BLOCKNAME[e])(lambda eng, e=e: body(eng, e))
        for e in ENGS:
            for o in ops[e][self.done[e]:]:
                o["fn"] = None if o["fn"] is None else True
            self.done[e] = len(ops[e])

    def close(self):
        self.pstack.close()


class Phase:
    def __init__(self, S):
        self.S = S
        self.stack = ExitStack()

    def sb(self, shape, dtype):
        self.S.n += 1
        return self.stack.enter_context(self.S.nc.sbuf_tensor(f"sb{self.S.n}", list(shape), dtype))

    def ps(self, shape=(128, 512), dtype=F32):
        self.S.n += 1
        return self.stack.enter_context(self.S.nc.psum_tensor(f"ps{self.S.n}", list(shape), dtype))

    def end(self):
        self.S.flush()
        self.stack.close()


def build_program(plan):
    nc = bass.Bass("TRN2", target_bir_lowering=False)
    FULL = dict(norm_mix=[4, D], norm_mem=[4, D], norm_ffn=[4, D], w_mem_kv=[4, D, 512], g_mq=[4, 64], g_mk=[4, 64],
                fox_w_in=[2, D, 2572], fox_b_f=[2, 12], fox_g_q=[2, 64], fox_g_k=[2, 64], fox_w_out=[2, D, D],
                conv_w_in=[2, D, 1792], conv_b_in=[2, 1536], conv_dw=[2, 31, 768], conv_dw_b=[2, 768], conv_ln_g=[2, 768],
                conv_ln_b=[2, 768], conv_w_out=[2, D, D], ffn_w_gate=[2, D, DFF], ffn_w_up=[2, D, DFF], ffn_w_down=[2, DFF, D],
                moe_router=[2, D, 8], moe_w_gate=[2, 8, D, EFF], moe_w_up=[2, 8, D, EFF], moe_w_down=[2, 8, EFF, D])
    declared = {}

    class LV:
        def __init__(self, name):
            self.name = name

        def _ap(self, idx, shape, row):
            nm = f"{self.name}_{idx}"
            if nm not in declared:
                declared[nm] = (nc.dram_tensor(nm, list(shape), F32, kind="ExternalInput").ap(), (self.name, idx, row))
            return declared[nm][0]

        def __getitem__(self, key):
            full = FULL[self.name]
            if isinstance(key, int):
                return self._ap(key, full[1:], False)
            if isinstance(key[0], slice):
                return self._ap(key[0].start, [1] + full[1:], True)
            return self._ap(key[0], full[1:], False)[key[1]]

    class LazyDin(dict):
        def __missing__(self, name):
            if name in FULL:
                v = LV(name)
            else:
                shape = dict(hin=[S_TOK, D], mem=[256, D], cst=[128, 512])[name]
                v = nc.dram_tensor(name, shape, F32, kind="ExternalInput").ap()
                declared[name] = (v, None)
            self[name] = v
            return v

    Din = LazyDin()
    out = nc.dram_tensor("out", [S_TOK, D], F32, kind="ExternalOutput").ap()
    hbuf = nc.dram_tensor("hbuf", [S_TOK, D], F32).ap()
    mixbuf = nc.dram_tensor("mixbuf", [8, 128, S_TOK], BF16).ap()
    r_h = regs(NT)
    r_mix = [regs(NTB) for _ in range(8)]

    S = Sched(nc)

    G = Phase(S)
    cst32 = G.sb([128, 512], F32); r_cst = Reg()
    cstb = G.sb([128, 512], BF16)
    epsT = G.sb([128, 1], F32); oneT = G.sb([128, 1], F32); zeroT = G.sb([128, 1], F32)
    onesF = G.sb([128, 128], F32); onesB = G.sb([128, 128], BF16)
    S.dma("sp", cst32[:], Din["cst"][:, :], writes=[r_cst], sem="cst")
    S.op("dve", lambda e: e.tensor_copy(cstb[:], cst32[:]), reads=[r_cst], writes=[r_cst])
    S.op("dve", lambda e: e.memset(epsT[:], EPS), writes=[r_cst])
    S.op("dve", lambda e: e.memset(oneT[:], 1.0), writes=[r_cst])
    S.op("dve", lambda e: e.memset(zeroT[:], 0.0), writes=[r_cst])
    S.op("dve", lambda e: e.memset(onesF[:], 1.0), writes=[r_cst])
    S.op("dve", lambda e: e.memset(onesB[:], 1.0), writes=[r_cst])
    identF = cst32[:, 0:128]; triF = cst32[:, 128:256]; selF = cst32[:, 384:512]
    identB = cstb[:, 0:128]; maskB = cstb[:, 128:256]; blkB = cstb[:, 256:384]
    S.flush()

    def rms_to_T(P, src_ap_fn, ntiles, gain_row_ap, dstT, r_dst_fn, pT, r_pT, tag):
        if not hasattr(P, "rms_tmp"):
            P.rms_tmp = (P.sb([128, D], F32), Reg(), [P.sb([128, D], F32) for _ in range(2)], regs(2),
                         P.sb([128, D], BF16), Reg(), [P.sb([128, 1], F32) for _ in range(2)], regs(2),
                         [P.sb([128, 1], F32) for _ in range(2)], regs(2), [P.sb([128, D], BF16) for _ in range(2)], regs(2))
        gbc, r_g, hl, r_hl, junk, r_junk, ss, r_ss, sd, r_sd, ub, r_ub = P.rms_tmp
        S.dma("sp", gbc[:], gain_row_ap.partition_broadcast(128), writes=[r_g], sem="rmsg")
        for t in range(ntiles):
            b = t % 2
            src, rsrc = src_ap_fn(t)
            S.dma("sp", hl[b][:], src, reads=rsrc, writes=[r_hl[b]], sem=f"{tag}h{b}")
            S.op("act", lambda e, b=b: e.activation(junk[:], hl[b][:], AF.Square, accum_out=ss[b][:, 0:1]),
                 reads=[r_hl[b]], writes=[r_junk, r_ss[b]])
            S.op("act", lambda e, b=b: e.activation(sd[b][:], ss[b][:], AF.Sqrt, bias=epsT[:, 0:1], scale=1.0 / D),
                 reads=[r_ss[b]], writes=[r_sd[b]])
            S.op("dve", lambda e, b=b: e.reciprocal(sd[b][:], sd[b][:]), reads=[r_sd[b]], writes=[r_sd[b]])
            S.op("dve", lambda e, b=b: e.scalar_tensor_tensor(ub[b][:], hl[b][:], sd[b][:, 0:1], gbc[:], ALU.mult, ALU.mult),
                 reads=[r_hl[b], r_sd[b], r_g], writes=[r_ub[b]])
            pt = pT[b]
            for k in range(8):
                S.op("pe", lambda e, b=b, k=k, pt=pt: e.transpose(pt[:, k * 128:(k + 1) * 128], ub[b][:, k * 128:(k + 1) * 128], identB),
                     reads=[r_ub[b], r_cst], writes=[r_pT[b]])
            eng = "act" if t % 2 == 0 else "dve"
            dst = dstT[:, :, t * 128:(t + 1) * 128]
            srcv = pt[:, :].rearrange("p (k n) -> p k n", k=8)
            if eng == "act":
                S.op("act", lambda e, dst=dst, srcv=srcv: e.copy(dst, srcv), reads=[r_pT[b]], writes=[r_dst_fn(t)])
            else:
                S.op("dve", lambda e, dst=dst, srcv=srcv: e.tensor_copy(dst, srcv), reads=[r_pT[b]], writes=[r_dst_fn(t)])

    def fm_norm(P, tmp, psrc, r_psrc, gcol, r_gcol, dst, r_dst, N, pS, r_pS, idx):
        sqb, r_sq, rs, r_rs = tmp
        b = idx % 2
        S.op("act", lambda e: e.activation(sqb[b][:, :N], psrc, AF.Square), reads=[r_psrc], writes=[r_sq[b]])
        S.op("pe", lambda e: e.matmul(pS[:, :N], blkB, sqb[b][:, :N], start=True, stop=True), reads=[r_sq[b], r_cst], writes=[r_pS])
        S.op("act", lambda e: e.activation(rs[b][:, :N], pS[:, :N], AF.Sqrt, bias=epsT[:, 0:1], scale=1.0 / 64),
             reads=[r_pS], writes=[r_rs[b]])
        S.op("dve", lambda e: e.reciprocal(rs[b][:, :N], rs[b][:, :N]), reads=[r_rs[b]], writes=[r_rs[b]])
        S.op("dve", lambda e: e.scalar_tensor_tensor(dst, psrc, gcol, rs[b][:, :N], ALU.mult, ALU.mult),
             reads=[r_psrc, r_rs[b], r_gcol], writes=[r_dst])

    def fm_tmp(P):
        return ([P.sb([128, 512], BF16) for _ in range(2)], regs(2), [P.sb([128, 512], F32) for _ in range(2)], regs(2))

    def load_gvec(P, rows, tag):
        n = len(rows)
        gv = P.sb([n, 128], F32); r_gv = Reg()
        for i, r in enumerate(rows):
            for hf in range(2):
                S.dma("sp", gv[i:i + 1, hf * 64:(hf + 1) * 64], r, writes=[r_gv], sem=tag)
        return gv, r_gv

    def mixer_phase(i, hin):
        j = i // 2
        fox = (i % 2 == 0)
        PO = Phase(S)
        if not fox:
            vt = PO.sb([36, 768], F32); r_vt = Reg()
            vT = PO.sb([128, 6, 36], F32); r_vT = Reg()
            ugT = PO.sb([128, 6, S_TOK + 32], BF16); r_ug = [regs(NTB) for _ in range(6)]
        P = Phase(S)
        uT = P.sb([128, 8, S_TOK], BF16); r_uT = regs(NTB)
        pA = [P.ps() for _ in range(2)]; r_pA = regs(2)
        pS = P.ps(); r_pS = Reg()
        pST = [P.ps() for _ in range(2)]; r_pST = regs(2)
        pO = [P.ps() for _ in range(2)]; r_pO = regs(2)
        pX = P.ps(); r_pX = Reg()
        pXb = pX[:, :].bitcast(BF16)
        pTb = [pXb, pO[0][:, :].bitcast(BF16)]
        rms_to_T(P, lambda t: (hin[t * 128:(t + 1) * 128, :], [r_h[t]]), NT, Din["norm_mix"][i:i + 1, :],
                 uT, lambda t: r_uT[t // 4], pTb, [r_pX, r_pO[0]], f"u{i}")
        tmp = fm_tmp(P)
        rows = ([Din["fox_g_q"][j:j + 1, :], Din["fox_g_k"][j:j + 1, :]] if fox else []) + \
               [Din["g_mq"][i:i + 1, :], Din["g_mk"][i:i + 1, :]]
        gv, r_gv = load_gvec(P, rows, f"gv{i}")
        gT = P.sb([128, 4], F32); r_gT = Reg()
        ng = len(rows)
        S.op("pe", lambda e: e.transpose(pS[:, 0:ng], gv[0:ng, :], identF[0:ng, 0:ng]), reads=[r_gv, r_cst], writes=[r_pS])
        S.op("dve", lambda e: e.tensor_copy(gT[:, 0:ng], pS[:, 0:ng]), reads=[r_pS], writes=[r_gT])
        c_gq, c_gk = (0, 1) if fox else (None, None)
        c_gmq, c_gmk = (2, 3) if fox else (0, 1)

        memT = P.sb([128, 8, 256], BF16); r_memT = Reg()
        rms_to_T(P, lambda t: (Din["mem"][t * 128:(t + 1) * 128, :], []), 2, Din["norm_mem"][i:i + 1, :],
                 memT, lambda t: r_memT, pTb, [r_pX, r_pO[0]], f"m{i}")
        wkv = P.sb([128, 8, 512], BF16); r_wkv = Reg()
        S.dma("pool", wkv[:], Din["w_mem_kv"][i].rearrange("(k p) n -> p k n", p=128), writes=[r_wkv], sem=f"wkv")
        kmT = P.sb([128, 2, 256], BF16); r_kmT = Reg()
        V1m = P.sb([128, 2, 4, 65], BF16); r_V1m = Reg()
        S.op("dve", lambda e: e.memset(V1m[:], 1.0), writes=[r_V1m])
        cnt = [0]

        def nxtA():
            cnt[0] += 1
            return pA[cnt[0] % 2], r_pA[cnt[0] % 2], cnt[0]

        for pr in range(2):
            p, rp, idx = nxtA()
            for k in range(8):
                S.op("pe", lambda e, p=p, k=k, pr=pr: e.matmul(p[:, 0:256], wkv[:, k, pr * 128:(pr + 1) * 128], memT[:, k, :], start=(k == 0), stop=(k == 7)),
                     reads=[r_wkv, r_memT], writes=[rp])
            fm_norm(P, tmp, p[:, 0:256], rp, gT[:, c_gmk:c_gmk + 1], r_gT, kmT[:, pr, :], r_kmT, 256, pS, r_pS, idx)
        for mt in range(2):
            p, rp, idx = nxtA()
            for k in range(8):
                S.op("pe", lambda e, p=p, k=k, mt=mt: e.matmul(p[:, 0:256], memT[:, k, mt * 128:(mt + 1) * 128], wkv[:, k, 256:512], start=(k == 0), stop=(k == 7)),
                     reads=[r_wkv, r_memT], writes=[rp])
            S.op("act", lambda e, p=p, mt=mt: e.copy(V1m[:, mt, :, 0:64], p[:, 0:256].rearrange("p (h d) -> p h d", h=4)),
                 reads=[rp], writes=[r_V1m])

        qT = P.sb([128, S_TOK], BF16); r_qT = regs(NTB)
        Pt = [P.sb([128, 512], BF16) for _ in range(3)]; r_Pt = regs(3)
        Osb = [P.sb([128, 512], F32) for _ in range(2)]; r_Osb = regs(2)
        rden = [P.sb([64, 512], F32) for _ in range(2)]; r_rden = regs(2)
        mixo = [P.sb([128, 512], BF16) for _ in range(2)]; r_mixo = regs(2)
        st = dict(p=0, o=0, m=0)

        def in_proj_fm(w, r_w, dstT, r_dstT, gcol):
            for tb in range(NTB):
                p, rp, idx = nxtA()
                for k in range(8):
                    S.op("pe", lambda e, p=p, k=k, tb=tb: e.matmul(p[:, :], w[:, k, :], uT[:, k, tb * 512:(tb + 1) * 512], start=(k == 0), stop=(k == 7)),
                         reads=[r_w, r_uT[tb]], writes=[rp])
                fm_norm(P, tmp, p[:, :], rp, gcol, r_gT, dstT[:, tb * 512:(tb + 1) * 512], r_dstT[tb], 512, pS, r_pS, idx)

        def finish_head(po, r_po, hp, qb, chunk, last):
            o = st["o"] % 2; st["o"] += 1
            m = st["m"] % 2
            S.op("act", lambda e: e.copy(Osb[o][0:65, :], po[0:65, :]), reads=[r_po], writes=[r_Osb[o]])
            S.op("pe", lambda e: e.matmul(pS[0:64, :], selF[0:65, 0:64], Osb[o][0:65, :], start=True, stop=True),
                 reads=[r_Osb[o], r_cst], writes=[r_pS])
            S.op("dve", lambda e: e.reciprocal(rden[o][:, :], pS[0:64, :]), reads=[r_pS], writes=[r_rden[o]])
            S.op("dve", lambda e: e.tensor_tensor(mixo[m][hp * 64:(hp + 1) * 64, :], Osb[o][0:64, :], rden[o][:, :], ALU.mult),
                 reads=[r_Osb[o], r_rden[o]], writes=[r_mixo[m]])
            if last:
                S.dma("sp", mixbuf[chunk, :, qb * 512:(qb + 1) * 512], mixo[m][:, :], reads=[r_mixo[m]], writes=[r_mix[chunk][qb]], sem=f"mixo{m}")
                st["m"] += 1

        def mem_attention(mqT, r_mqT, pr):
            for qb in range(NTB):
                for hp in range(2):
                    po = pO[st["o"] % 2]; r_po = r_pO[st["o"] % 2]
                    for mt in range(2):
                        pi = st["p"] % 3; st["p"] += 1
                        ps_, rps = pST[pi % 2], r_pST[pi % 2]
                        S.op("pe", lambda e, ps_=ps_, mt=mt, hp=hp, qb=qb: e.matmul(ps_[:, :], kmT[hp * 64:(hp + 1) * 64, pr, mt * 128:(mt + 1) * 128],
                                                                    mqT[hp * 64:(hp + 1) * 64, qb * 512:(qb + 1) * 512], start=True, stop=True),
                             reads=[r_kmT, r_mqT[qb]], writes=[rps])
                        S.op("act", lambda e, ps_=ps_, pi=pi: e.activation(Pt[pi][:, :], ps_[:, :], AF.Exp, bias=zeroT[:, 0:1], scale=0.125),
                             reads=[rps], writes=[r_Pt[pi]])
                        S.op("pe", lambda e, po=po, pi=pi, mt=mt, hp=hp: e.matmul(po[0:65, :], V1m[:, mt, 2 * pr + hp, :], Pt[pi][:, :], start=(mt == 0), stop=(mt == 1)),
                             reads=[r_V1m, r_Pt[pi]], writes=[r_po])
                    finish_head(po, r_po, hp, qb, 6 + pr, hp == 1)

        Win = Din["fox_w_in"][j] if fox else Din["conv_w_in"][j]
        wq = P.sb([128, 8, 128], BF16); r_wq = Reg()
        wk = P.sb([128, 8, 128], BF16); r_wk = Reg()
        wv = P.sb([128, 8, 128], BF16); r_wv = Reg()

        def load_w(dst, r_dst, col0, ncol, sem):
            S.dma("pool", dst[:, :, 0:ncol], Win[:, col0:col0 + ncol].rearrange("(k p) n -> p k n", p=128), writes=[r_dst], sem=sem)

        if fox:
            kT = P.sb([128, S_TOK], BF16); r_kT = regs(NTB)
            V1 = P.sb([128, NT, 2, 65], BF16); r_V1 = regs(NTB)
            S.op("dve", lambda e: e.memset(V1[:], 1.0), writes=r_V1)
            wf = P.sb([128, 8, 12], BF16); r_wf = Reg()
            load_w(wf, r_wf, 2304, 12, "wf")
            bfb = P.sb([128, 12], F32); r_bfb = Reg()
            S.dma("sp", bfb[:], Din["fox_b_f"][j:j + 1, :].partition_broadcast(128), writes=[r_bfb], sem="bfb")
            Ctok = P.sb([128, 12, NT], F32); r_C = Reg()
            carry = P.sb([128, 12, NT + 1], F32); r_carry = Reg()
            S.op("dve", lambda e: e.memset(carry[:], 0.0), writes=[r_carry])
            nl = [P.sb([128, 12], F32) for _ in range(2)]; r_nl = regs(2)
            tl = [P.sb([128, 12], F32) for _ in range(2)]; r_tl = regs(2)
            for t in range(NT):
                b = t % 2
                p, rp, idx = nxtA()
                for k in range(8):
                    S.op("pe", lambda e, p=p, k=k, t=t: e.matmul(p[:, 0:12], uT[:, k, t * 128:(t + 1) * 128], wf[:, k, :], start=(k == 0), stop=(k == 7)),
                         reads=[r_wf, r_uT[t // 4]], writes=[rp])
                S.op("dve", lambda e, p=p, b=b: e.tensor_tensor(tl[b][:], p[:, 0:12], bfb[:], ALU.add), reads=[rp, r_bfb], writes=[r_tl[b]])
                S.op("act", lambda e, b=b: e.activation(tl[b][:], tl[b][:], AF.Exp, bias=zeroT[:, 0:1], scale=-1.0), reads=[r_tl[b]], writes=[r_tl[b]])
                S.op("act", lambda e, b=b: e.activation(nl[b][:], tl[b][:], AF.Ln, bias=oneT[:, 0:1], scale=1.0), reads=[r_tl[b]], writes=[r_nl[b]])
                S.op("pe", lambda e, b=b: e.matmul(pS[:, 0:12], triF, nl[b][:], start=True, stop=True), reads=[r_nl[b], r_cst], writes=[r_pS])
                S.op("pe", lambda e, b=b: e.matmul(pS[:, 16:28], onesF[:, :], nl[b][:], start=True, stop=True), reads=[r_nl[b], r_cst], writes=[r_pS])
                S.op("dve", lambda e, t=t: e.tensor_tensor(Ctok[:, :, t], pS[:, 0:12], carry[:, :, t], ALU.add), reads=[r_pS, r_carry], writes=[r_C])
                S.op("dve", lambda e, t=t: e.tensor_tensor(carry[:, :, t + 1], pS[:, 16:28], carry[:, :, t], ALU.add), reads=[r_pS, r_carry], writes=[r_carry])
            Bq = [P.sb([128, 4], F32) for _ in range(2)]; r_Bq = regs(2)
            nb = [0]
            for pj in range(6):
                load_w(wq, r_wq, pj * 128, 128, "wq")
                load_w(wk, r_wk, 768 + pj * 128, 128, "wk")
                load_w(wv, r_wv, 1536 + pj * 128, 128, "wv")
                in_proj_fm(wq, r_wq, qT, r_qT, gT[:, c_gq:c_gq + 1])
                in_proj_fm(wk, r_wk, kT, r_kT, gT[:, c_gk:c_gk + 1])
                for t in range(NT):
                    p, rp, idx = nxtA()
                    for k in range(8):
                        S.op("pe", lambda e, p=p, k=k, t=t: e.matmul(p[:, 0:128], uT[:, k, t * 128:(t + 1) * 128], wv[:, k, :], start=(k == 0), stop=(k == 7)),
                             reads=[r_wv, r_uT[t // 4]], writes=[rp])
                    ev = "act" if t % 2 == 0 else "dve"
                    dst = V1[:, t, :, 0:64]
                    srcv = p[:, 0:128].rearrange("p (h d) -> p h d", h=2)
                    if ev == "act":
                        S.op("act", lambda e, dst=dst, srcv=srcv: e.copy(dst, srcv), reads=[rp], writes=[r_V1[t // 4]])
                    else:
                        S.op("dve", lambda e, dst=dst, srcv=srcv: e.tensor_copy(dst, srcv), reads=[rp], writes=[r_V1[t // 4]])
                for qb in range(NTB):
                    for hp in range(2):
                        h = 2 * pj + hp
                        po = pO[st["o"] % 2]; r_po = r_pO[st["o"] % 2]
                        nk = 4 * qb + 4
                        for kt in range(nk):
                            pi = st["p"] % 3; st["p"] += 1
                            ps_, rps = pST[pi % 2], r_pST[pi % 2]
                            bq = nb[0] % 2; nb[0] += 1
                            S.op("pe", lambda e, ps_=ps_, kt=kt, hp=hp, qb=qb: e.matmul(ps_[:, :], kT[hp * 64:(hp + 1) * 64, kt * 128:(kt + 1) * 128],
                                                                                      qT[hp * 64:(hp + 1) * 64, qb * 512:(qb + 1) * 512], start=True, stop=True),
                                 reads=[r_kT[kt // 4], r_qT[qb]], writes=[rps])
                            S.op("dve", lambda e, bq=bq, h=h, qb=qb, kt=kt: e.tensor_scalar(Bq[bq][:, :], carry[:, h, 4 * qb:4 * qb + 4], Ctok[:, h, kt:kt + 1], -1.0, ALU.subtract, ALU.mult),
                                 reads=[r_carry, r_C], writes=[r_Bq[bq]])
                            r = kt - 4 * qb
                            for jj in range(4):
                                if r > jj:
                                    continue
                                S.op("act", lambda e, ps_=ps_, pi=pi, jj=jj, bq=bq: e.activation(Pt[pi][:, jj * 128:(jj + 1) * 128], ps_[:, jj * 128:(jj + 1) * 128], AF.Exp,
                                                                                                bias=Bq[bq][:, jj:jj + 1], scale=0.125),
                                     reads=[rps, r_Bq[bq]], writes=[r_Pt[pi]])
                            if r >= 0:
                                S.op("dve", lambda e, pi=pi, r=r: e.tensor_tensor(Pt[pi][:, r * 128:(r + 1) * 128], Pt[pi][:, r * 128:(r + 1) * 128], maskB, ALU.mult),
                                     reads=[r_Pt[pi], r_cst], writes=[r_Pt[pi]])
                                if r > 0:
                                    S.op("dve", lambda e, pi=pi, r=r: e.memset(Pt[pi][:, 0:r * 128], 0.0), writes=[r_Pt[pi]])
                            S.op("pe", lambda e, po=po, pi=pi, kt=kt, hp=hp, nk=nk: e.matmul(po[0:65, :], V1[:, kt, hp, :], Pt[pi][:, :], start=(kt == 0), stop=(kt == nk - 1)),
                                 reads=[r_V1[kt // 4], r_Pt[pi]], writes=[r_po])
                        finish_head(po, r_po, hp, qb, pj, hp == 1)
            for pr in range(2):
                load_w(wq, r_wq, 2316 + pr * 128, 128, "wq")
                in_proj_fm(wq, r_wq, qT, r_qT, gT[:, c_gmq:c_gmq + 1])
                mem_attention(qT, r_qT, pr)
        else:
            S.dma("sp", vt[0:31, :], Din["conv_dw"][j], writes=[r_vt], sem="vt")
            S.dma("sp", vt[31:32, :], Din["conv_dw_b"][j:j + 1, :], writes=[r_vt], sem="vt")
            S.dma("sp", vt[32:33, :], Din["conv_ln_g"][j:j + 1, :], writes=[r_vt], sem="vt")
            S.dma("sp", vt[33:34, :], Din["conv_ln_b"][j:j + 1, :], writes=[r_vt], sem="vt")
            S.dma("sp", vt[34:36, :], Din["conv_b_in"][j].rearrange("(a n) -> a n", a=2), writes=[r_vt], sem="vt")
            for c in range(6):
                S.op("pe", lambda e, c=c: e.transpose(pS[:, 0:36], vt[0:36, c * 128:(c + 1) * 128], identF[0:36, 0:36]), reads=[r_vt, r_cst], writes=[r_pS])
                S.op("dve", lambda e, c=c: e.tensor_copy(vT[:, c, :], pS[:, 0:36]), reads=[r_pS], writes=[r_vT])
            S.op("dve", lambda e: e.memset(ugT[:, :, 0:32], 0.0), writes=[r_ug[c][0] for c in range(6)])
            sgm = [P.sb([128, 512], F32) for _ in range(2)]; r_sgm = regs(2)
            OFF = 32
            for c in range(6):
                load_w(wq, r_wq, c * 128, 128, "wq")
                load_w(wk, r_wk, 768 + c * 128, 128, "wk")
                for tb in range(NTB):
                    pa, rpa, _ = nxtA()
                    pb, rpb, idx = nxtA()
                    for k in range(8):
                        S.op("pe", lambda e, pa=pa, k=k, tb=tb: e.matmul(pa[:, :], wq[:, k, :], uT[:, k, tb * 512:(tb + 1) * 512], start=(k == 0), stop=(k == 7)),
                             reads=[r_wq, r_uT[tb]], writes=[rpa])
                    for k in range(8):
                        S.op("pe", lambda e, pb=pb, k=k, tb=tb: e.matmul(pb[:, :], wk[:, k, :], uT[:, k, tb * 512:(tb + 1) * 512], start=(k == 0), stop=(k == 7)),
                             reads=[r_wk, r_uT[tb]], writes=[rpb])
                    b = idx % 2
                    S.op("act", lambda e, pb=pb, b=b, c=c: e.activation(sgm[b][:, :], pb[:, :], AF.Sigmoid, bias=vT[:, c, 35:36], scale=1.0),
                         reads=[rpb, r_vT], writes=[r_sgm[b]])
                    S.op("dve", lambda e, pa=pa, b=b, c=c, tb=tb: e.scalar_tensor_tensor(ugT[:, c, OFF + tb * 512:OFF + (tb + 1) * 512], pa[:, :], vT[:, c, 34:35], sgm[b][:, :], ALU.add, ALU.mult),
                         reads=[rpa, r_sgm[b], r_vT], writes=[r_ug[c][tb]])
            for pr in range(2):
                load_w(wv, r_wv, 1536 + pr * 128, 128, "wv")
                in_proj_fm(wv, r_wv, qT, r_qT, gT[:, c_gmq:c_gmq + 1])
                mem_attention(qT, r_qT, pr)
            P.end()
            P = Phase(S)
            xc = [P.sb([128, 6, 512], F32) for _ in range(2)]; r_xc = [regs(6) for _ in range(2)]
            sq = P.sb([128, 512], F32); r_sqx = Reg()
            mean = P.sb([128, 512], F32); r_mean = Reg()
            rstd = P.sb([128, 512], F32); r_rstd = Reg()
            t1 = [P.sb([128, 512], F32) for _ in range(2)]; r_t1 = regs(2)
            mixo = [P.sb([128, 512], BF16) for _ in range(2)]; r_mixo = regs(2)
            pM, r_pM = P.ps(), Reg()
            pV, r_pV = P.ps(), Reg()
            for tb in range(NTB):
                xb = tb % 2
                for c in range(6):
                    acc = xc[xb][:, c, :]
                    rd = [r_ug[c][tb]] + ([r_ug[c][tb - 1]] if tb > 0 else []) + [r_vT]
                    base = OFF + tb * 512 - 30
                    S.op("dve", lambda e, acc=acc, c=c, base=base: e.tensor_scalar(acc, ugT[:, c, base:base + 512], vT[:, c, 0:1], vT[:, c, 31:32], ALU.mult, ALU.add),
                         reads=rd, writes=[r_xc[xb][c]])
                    for w in range(1, 31):
                        S.op("dve", lambda e, acc=acc, c=c, base=base, w=w: e.scalar_tensor_tensor(acc, ugT[:, c, base + w:base + w + 512], vT[:, c, w:w + 1], acc, ALU.mult, ALU.add),
                             reads=rd, writes=[r_xc[xb][c]])
                for c in range(6):
                    S.op("pe", lambda e, c=c, xb=xb: e.matmul(pM[:, :], onesF[:, :], xc[xb][:, c, :], start=(c == 0), stop=(c == 5)),
                         reads=[r_xc[xb][c], r_cst], writes=[r_pM])
                for c in range(6):
                    S.op("act", lambda e, c=c, xb=xb: e.activation(sq[:, :], xc[xb][:, c, :], AF.Square), reads=[r_xc[xb][c]], writes=[r_sqx])
                    S.op("pe", lambda e, c=c: e.matmul(pV[:, :], onesF[:, :], sq[:, :], start=(c == 0), stop=(c == 5)),
                         reads=[r_sqx, r_cst], writes=[r_pV])
                S.op("dve", lambda e: e.tensor_scalar(mean[:, :], pM[:, :], 1.0 / 768, None, ALU.mult), reads=[r_pM], writes=[r_mean])
                S.op("dve", lambda e: e.tensor_tensor(rstd[:, :], mean[:, :], mean[:, :], ALU.mult), reads=[r_mean], writes=[r_rstd])
                S.op("dve", lambda e: e.scalar_tensor_tensor(rstd[:, :], pV[:, :], 1.0 / 768, rstd[:, :], ALU.mult, ALU.subtract), reads=[r_pV, r_rstd], writes=[r_rstd])
                S.op("act", lambda e: e.activation(rstd[:, :], rstd[:, :], AF.Sqrt, bias=epsT[:, 0:1], scale=1.0), reads=[r_rstd], writes=[r_rstd])
                S.op("dve", lambda e: e.reciprocal(rstd[:, :], rstd[:, :]), reads=[r_rstd], writes=[r_rstd])
                for c in range(6):
                    tt = c % 2
                    m = st["m"] % 2; st["m"] += 1
                    S.op("dve", lambda e, c=c, xb=xb, tt=tt: e.tensor_tensor(t1[tt][:, :], xc[xb][:, c, :], mean[:, :], ALU.subtract), reads=[r_xc[xb][c], r_mean], writes=[r_t1[tt]])
                    S.op("dve", lambda e, tt=tt: e.tensor_tensor(t1[tt][:, :], t1[tt][:, :], rstd[:, :], ALU.mult), reads=[r_t1[tt], r_rstd], writes=[r_t1[tt]])
                    S.op("act", lambda e, c=c, tt=tt, m=m: e.activation(mixo[m][:, :], t1[tt][:, :], AF.Silu, bias=vT[:, c, 33:34], scale=vT[:, c, 32:33]),
                         reads=[r_t1[tt], r_vT], writes=[r_mixo[m]])
                    S.dma("sp", mixbuf[c, :, tb * 512:(tb + 1) * 512], mixo[m][:, :], reads=[r_mixo[m]], writes=[r_mix[c][tb]], sem=f"mixo{m}")
        P.end()
        PO.stack.close()

    def outproj_phase(i, hin, hout):
        j = i // 2
        Wo = Din["fox_w_out"][j] if i % 2 == 0 else Din["conv_w_out"][j]
        P = Phase(S)
        wout = P.sb([128, 8, D], BF16); r_wo = Reg()
        S.dma("pool", wout[:], Wo.rearrange("(k p) n -> p k n", p=128), writes=[r_wo], sem="wout")
        mixl = [P.sb([128, 8, 512], BF16) for _ in range(2)]; r_ml = regs(2)
        hl = [P.sb([128, D], F32) for _ in range(3)]; r_hl = regs(3)
        pd = [[P.ps() for _ in range(2)] for _ in range(2)]; r_pd = [regs(2) for _ in range(2)]
        for tb in range(NTB):
            mb = tb % 2
            S.dma("sp", mixl[mb][:], mixbuf[:, :, tb * 512:(tb + 1) * 512].rearrange("c p n -> p c n"),
                  reads=[r_mix[c][tb] for c in range(8)], writes=[r_ml[mb]], sem=f"ml{mb}")
            for tt in range(4):
                t = tb * 4 + tt
                hb = t % 3
                pb = t % 2
                S.dma("sp", hl[hb][:], hin[t * 128:(t + 1) * 128, :], reads=[r_h[t]], writes=[r_hl[hb]], sem=f"oh{hb}")
                for half in range(2):
                    for c in range(8):
                        S.op("pe", lambda e, pb=pb, half=half, c=c, mb=mb, tt=tt: e.matmul(pd[pb][half][:, :], mixl[mb][:, c, tt * 128:(tt + 1) * 128], wout[:, c, half * 512:(half + 1) * 512],
                                                                                          start=(c == 0), stop=(c == 7)),
                             reads=[r_ml[mb], r_wo], writes=[r_pd[pb][half]])
                    S.op("dve", lambda e, pb=pb, half=half, hb=hb: e.tensor_tensor(hl[hb][:, half * 512:(half + 1) * 512], pd[pb][half][:, :], hl[hb][:, half * 512:(half + 1) * 512], ALU.add),
                         reads=[r_pd[pb][half], r_hl[hb]], writes=[r_hl[hb]])
                S.dma("sp", hout[t * 128:(t + 1) * 128, :], hl[hb][:], reads=[r_hl[hb]], writes=[r_h[t]], sem=f"os{hb}")
        P.end()

    def ffn_phase(i, hin, hout, sbks=(0, 1)):
        j = i // 2
        moe = (i % 2 == 1)
        NE = NEXP if moe else 1
        FF = EFF if moe else DFF
        GC = 4 if moe else 2
        NCG = FF // 128 // GC
        NSB = 2
        TSB = S_TOK // NSB
        NTS = TSB // 128
        for sbk in sbks:
            P = Phase(S)
            acc = P.sb([128, NTS, D], F32); r_acc = regs(NTS)
            zT = P.sb([128, 8, TSB], BF16); r_zT = regs(TSB // 256)
            gates = P.sb([128, NTS, 8], F32); r_gates = regs(NTS)
            gbc = P.sb([128, D], F32); r_g = Reg()
            S.dma("sp", gbc[:], Din["norm_ffn"][i:i + 1, :].partition_broadcast(128), writes=[r_g], sem="fg")
            wg = [P.sb([128, 8, GC * 128], BF16) for _ in range(2)]; r_wg = regs(2)
            wu = [P.sb([128, 8, GC * 128], BF16) for _ in range(2)]; r_wu = regs(2)
            wd = [P.sb([128, GC, D], BF16) for _ in range(2)]; r_wd = regs(2)
            aT = [P.sb([128, GC, 256], BF16) for _ in range(2)]; r_aT = regs(2)
            sg = [P.sb([128, 256], F32) for _ in range(3)]; r_sg = regs(3)
            pd = [[P.ps() for _ in range(2)] for _ in range(2)]; r_pd = [regs(2) for _ in range(2)]
            pg = [P.ps() for _ in range(2)]; r_pg = regs(2)
            pu = [P.ps() for _ in range(2)]; r_pu = regs(2)
            junk = P.sb([128, D], BF16); r_junk = Reg()
            ss = [P.sb([128, 1], F32) for _ in range(2)]; r_ss = regs(2)
            sd = [P.sb([128, 1], F32) for _ in range(2)]; r_sd = regs(2)
            z32 = [P.sb([128, D], F32) for _ in range(2)]; r_z32 = regs(2)
            if moe:
                wr = P.sb([128, 8, 8], F32); r_wr = Reg()
                wrA = P.sb([8, 1024], F32); r_wrA = Reg()
                S.dma("sp", wrA[:], Din["moe_router"][j].rearrange("(i r) n -> i (r n)", i=8), writes=[r_wrA], sem="wr")
                wrAv = wrA[0:8, :].rearrange("i (r n) -> i r n", n=8)
                for n in range(8):
                    S.op("pe", lambda e, n=n: e.transpose(pg[0][:, n * 8:(n + 1) * 8], wrAv[:, :, n], identF[0:8, 0:8]), reads=[r_wrA, r_cst], writes=[r_pg[0]])
                S.op("act", lambda e: e.copy(wr[:, :, :].rearrange("p k n -> p n k"), pg[0][:, 0:64].rearrange("p (n k) -> p n k", n=8)), reads=[r_pg[0]], writes=[r_wr])
                zT32 = [P.sb([128, 8, 128], F32) for _ in range(2)]; r_zT32 = regs(2)
                rt = {n: [P.sb([128, 8], F32) for _ in range(2)] for n in ("L", "eq", "L2", "sel", "ex")}
                rc = {n: [P.sb([128, 1], F32) for _ in range(2)] for n in ("m1", "m2", "nm1", "den")}
                r_rt = regs(2)
            for t in range(NTS):
                b = t % 2
                tg = sbk * NTS + t
                S.dma("sp", acc[:, t, :], hin[tg * 128:(tg + 1) * 128, :], reads=[r_h[tg]], writes=[r_acc[t]], sem=f"fh{b}")
                S.op("act", lambda e, b=b, t=t: e.activation(junk[:], acc[:, t, :], AF.Square, accum_out=ss[b][:, 0:1]),
                     reads=[r_acc[t]], writes=[r_junk, r_ss[b]])
                S.op("act", lambda e, b=b: e.activation(sd[b][:], ss[b][:], AF.Sqrt, bias=epsT[:, 0:1], scale=1.0 / D), reads=[r_ss[b]], writes=[r_sd[b]])
                S.op("dve", lambda e, b=b: e.reciprocal(sd[b][:], sd[b][:]), reads=[r_sd[b]], writes=[r_sd[b]])
                S.op("dve", lambda e, b=b, t=t: e.scalar_tensor_tensor(z32[b][:], acc[:, t, :], sd[b][:, 0:1], gbc[:], ALU.mult, ALU.mult),
                     reads=[r_acc[t], r_sd[b], r_g], writes=[r_z32[b]])
                for hf in range(2):
                    pt, rpt = pg[hf], r_pg[hf]
                    for kk in range(4):
                        k = hf * 4 + kk
                        S.op("pe", lambda e, pt=pt, kk=kk, k=k, b=b: e.transpose(pt[:, kk * 128:(kk + 1) * 128], z32[b][:, k * 128:(k + 1) * 128], identF),
                             reads=[r_z32[b], r_cst], writes=[rpt])
                    S.op("act", lambda e, pt=pt, hf=hf, t=t: e.copy(zT[:, hf * 4:(hf + 1) * 4, t * 128:(t + 1) * 128], pt[:, :].rearrange("p (k n) -> p k n", k=4)),
                         reads=[rpt], writes=[r_zT[t // 2]])
                    if moe:
                        S.op("act", lambda e, pt=pt, hf=hf, b=b: e.copy(zT32[b][:, hf * 4:(hf + 1) * 4, :], pt[:, :].rearrange("p (k n) -> p k n", k=4)),
                             reads=[rpt], writes=[r_zT32[b]])
                if moe:
                    pl, rpl = pu[b], r_pu[b]
                    for k in range(8):
                        S.op("pe", lambda e, pl=pl, k=k, b=b: e.matmul(pl[:, 0:8], zT32[b][:, k, :], wr[:, k, :], start=(k == 0), stop=(k == 7)),
                             reads=[r_zT32[b], r_wr], writes=[rpl])
                    L, eq, L2, sel, ex = (rt[n][b] for n in ("L", "eq", "L2", "sel", "ex"))
                    m1, m2, nm1, den = (rc[n][b] for n in ("m1", "m2", "nm1", "den"))
                    R = [r_rt[b]]
                    S.op("dve", lambda e, L=L, pl=pl: e.tensor_copy(L[:], pl[:, 0:8]), reads=[rpl], writes=R)
                    S.op("dve", lambda e, L=L, m1=m1: e.tensor_reduce(m1[:], L[:], AX.X, ALU.max), reads=R, writes=R)
                    S.op("dve", lambda e, L=L, m1=m1, eq=eq: e.tensor_scalar(eq[:], L[:], m1[:, 0:1], None, ALU.is_ge), reads=R, writes=R)
                    S.op("dve", lambda e, L=L, L2=L2, eq=eq: e.scalar_tensor_tensor(L2[:], eq[:], -1e30, L[:], ALU.mult, ALU.add), reads=R, writes=R)
                    S.op("dve", lambda e, L2=L2, m2=m2: e.tensor_reduce(m2[:], L2[:], AX.X, ALU.max), reads=R, writes=R)
                    S.op("dve", lambda e, L=L, m2=m2, sel=sel: e.tensor_scalar(sel[:], L[:], m2[:, 0:1], None, ALU.is_ge), reads=R, writes=R)
                    S.op("dve", lambda e, m1=m1, nm1=nm1: e.tensor_scalar(nm1[:], m1[:], -1.0, None, ALU.mult), reads=R, writes=R)
                    S.op("act", lambda e, L=L, ex=ex, nm1=nm1: e.activation(ex[:], L[:], AF.Exp, bias=nm1[:, 0:1], scale=1.0), reads=R, writes=R)
                    S.op("dve", lambda e, ex=ex, sel=sel: e.tensor_tensor(ex[:], ex[:], sel[:], ALU.mult), reads=R, writes=R)
                    S.op("dve", lambda e, ex=ex, den=den: e.tensor_reduce(den[:], ex[:], AX.X, ALU.add), reads=R, writes=R)
                    S.op("dve", lambda e, den=den: e.reciprocal(den[:], den[:]), reads=R, writes=R)
                    S.op("dve", lambda e, ex=ex, den=den, t=t: e.tensor_scalar(gates[:, t, :], ex[:], den[:, 0:1], None, ALU.mult), reads=R, writes=[r_gates[t]])
            NTB2 = TSB // 256
            gi = 0
            for ex_ in range(NE):
                if moe:
                    Wg, Wu, Wd = Din["moe_w_gate"][j, ex_], Din["moe_w_up"][j, ex_], Din["moe_w_down"][j, ex_]
                else:
                    Wg, Wu, Wd = Din["ffn_w_gate"][j], Din["ffn_w_up"][j], Din["ffn_w_down"][j]
                for cg in range(NCG):
                    s = gi % 2; gi += 1
                    c0 = cg * GC * 128
                    S.dma("pool", wg[s][:], Wg[:, c0:c0 + GC * 128].rearrange("(k p) n -> p k n", p=128), writes=[r_wg[s]], sem=f"wg{s}")
                    S.dma("pool", wu[s][:], Wu[:, c0:c0 + GC * 128].rearrange("(k p) n -> p k n", p=128), writes=[r_wu[s]], sem=f"wu{s}")
                    S.dma("pool", wd[s][:], Wd[c0:c0 + GC * 128, :].rearrange("(c p) n -> p c n", p=128), writes=[r_wd[s]], sem=f"wd{s}")

                    def stageA(tb, s=s):
                        ab = tb % 2
                        for c in range(GC):
                            q = (tb * GC + c) % 2
                            for k in range(8):
                                S.op("pe", lambda e, q=q, c=c, k=k, tb=tb, s=s: e.matmul(pg[q][:, 0:256], wg[s][:, k, c * 128:(c + 1) * 128], zT[:, k, tb * 256:(tb + 1) * 256], start=(k == 0), stop=(k == 7)),
                                     reads=[r_wg[s], r_zT[tb]], writes=[r_pg[q]])
                            for k in range(8):
                                S.op("pe", lambda e, q=q, c=c, k=k, tb=tb, s=s: e.matmul(pu[q][:, 0:256], wu[s][:, k, c * 128:(c + 1) * 128], zT[:, k, tb * 256:(tb + 1) * 256], start=(k == 0), stop=(k == 7)),
                                     reads=[r_wu[s], r_zT[tb]], writes=[r_pu[q]])
                            g3 = (tb * GC + c) % 3
                            S.op("act", lambda e, q=q, g3=g3: e.activation(sg[g3][:, :], pg[q][:, 0:256], AF.Silu), reads=[r_pg[q]], writes=[r_sg[g3]])
                            S.op("dve", lambda e, q=q, g3=g3, ab=ab, c=c: e.tensor_tensor(aT[ab][:, c, :], pu[q][:, 0:256], sg[g3][:, :], ALU.mult),
                                 reads=[r_pu[q], r_sg[g3]], writes=[r_aT[ab]])

                    def stageB(tb, s=s, ex_=ex_):
                        ab = tb % 2
                        for tt in range(2):
                            t = tb * 2 + tt
                            for half in range(2):
                                for c in range(GC):
                                    S.op("pe", lambda e, tt=tt, half=half, c=c, ab=ab, s=s: e.matmul(pd[tt][half][:, :], aT[ab][:, c, tt * 128:(tt + 1) * 128], wd[s][:, c, half * 512:(half + 1) * 512],
                                                                                                    start=(c == 0), stop=(c == GC - 1)),
                                         reads=[r_aT[ab], r_wd[s]], writes=[r_pd[tt][half]])
                                dst = acc[:, t, half * 512:(half + 1) * 512]
                                if moe:
                                    S.op("dve", lambda e, tt=tt, half=half, dst=dst, t=t, ex_=ex_: e.scalar_tensor_tensor(dst, pd[tt][half][:, :], gates[:, t, ex_:ex_ + 1], dst, ALU.mult, ALU.add),
                                         reads=[r_pd[tt][half], r_gates[t], r_acc[t]], writes=[r_acc[t]])
                                else:
                                    S.op("dve", lambda e, tt=tt, half=half, dst=dst: e.tensor_tensor(dst, pd[tt][half][:, :], dst, ALU.add),
                                         reads=[r_pd[tt][half], r_acc[t]], writes=[r_acc[t]])

                    stageA(0)
                    for tb in range(NTB2):
                        if tb + 1 < NTB2:
                            stageA(tb + 1)
                        stageB(tb)
            for t in range(NTS):
                tg = sbk * NTS + t
                S.dma("sp", hout[tg * 128:(tg + 1) * 128, :], acc[:, t, :], reads=[r_acc[t]], writes=[r_h[tg]], sem="fst")
            P.end()

    hin = Din["hin"]
    for pi, ph in enumerate(plan):
        last = (pi == len(plan) - 1)
        hout = out if last else hbuf
        if ph[0] == "mix":
            mixer_phase(ph[1], hin)
        elif ph[0] == "out":
            outproj_phase(ph[1], hin, hout)
            hin = hout
        else:
            ffn_phase(ph[1], hin, hout, ph[2])
            hin = hout
    G.stack.close()
    S.close()
    return nc, {k: v[1] for k, v in declared.items()}


def make_consts():
    c = np.zeros((128, 512), np.float32)
    c[:, 0:128] = np.eye(128, dtype=np.float32)
    c[:, 128:256] = np.triu(np.ones((128, 128), np.float32))
    c[0:64, 256:320] = 1.0
    c[64:128, 320:384] = 1.0
    c[64, 384:512] = 1.0
    return c


_WEIGHT_KEYS = ["norm_mix", "norm_mem", "norm_ffn", "w_mem_kv", "g_mq", "g_mk", "fox_w_in", "fox_b_f", "fox_g_q", "fox_g_k",
                "fox_w_out", "conv_w_in", "conv_b_in", "conv_dw", "conv_dw_b", "conv_ln_g", "conv_ln_b", "conv_w_out",
                "ffn_w_gate", "ffn_w_up", "ffn_w_down", "moe_router", "moe_w_gate", "moe_w_up", "moe_w_down"]


def _full_plan():
    plan = []
    for i in range(4):
        plan += [("mix", i), ("out", i), ("ffn", i, (0, 1))]
    return plan


LAUNCHES = [_full_plan()]


def _launch(plan, inputs, h):
    nc, declared = build_program(plan)
    shared = {}
    for name, info in declared.items():
        if name in ("hin", "mem"):
            continue
        if name == "cst":
            shared[name] = make_consts()
            continue
        base, idx, row = info
        a = np.asarray(inputs[base][idx], dtype=np.float32)
        shared[name] = np.ascontiguousarray(a[None] if row else a)
    mem = np.asarray(inputs["mem"], dtype=np.float32)
    in_maps = []
    for b in range(8):
        m = dict(shared)
        m["hin"] = np.ascontiguousarray(h[b])
        if "mem" in declared:
            m["mem"] = np.ascontiguousarray(mem[b])
        in_maps.append(m)
    res = run_bass_kernel_spmd(nc, in_maps, core_ids=list(range(8)))
    return np.stack([np.asarray(r["out"], dtype=np.float32) for r in res.results], axis=0)


def kernel(**inputs):
    h = np.asarray(inputs["x"], dtype=np.float32)
    for plan in LAUNCHES:
        h = _launch(plan, inputs, h)
    return h
```

```python
from contextlib import ExitStack
import numpy as np
import concourse.bass as bass
import concourse.mybir as mybir
from concourse.bass_utils import run_bass_kernel_spmd

F32 = mybir.dt.float32
BF16 = mybir.dt.bfloat16
AF = mybir.ActivationFunctionType
ALU = mybir.AluOpType
AX = mybir.AxisListType

ENGS = ["pe", "act", "dve", "pool", "sp"]
BLOCKNAME = {"pe": "tensor", "act": "scalar", "dve": "vector", "pool": "gpsimd", "sp": "sync"}

S_TOK = 4096
D = 1024
NT = S_TOK // 128
NTB = S_TOK // 512
EPS = 1e-6
DFF = 2816
EFF = 3584
NEXP = 8


class Reg:
    __slots__ = ("w", "rs")

    def __init__(self):
        self.w = None
        self.rs = {}


def regs(n):
    return [Reg() for _ in range(n)]


class Sched:
    def __init__(self, nc):
        self.nc = nc
        self.ops = {e: [] for e in ENGS}
        self.done = {e: 0 for e in ENGS}
        self.cnt = {e: 0 for e in ENGS}
        self.waited = {e: {} for e in ENGS}
        self.dma_cnt = []
        self.dma_names = {}
        self.pstack = ExitStack()
        self.esem = {e: self.pstack.enter_context(nc.semaphore(f"es_{e}")) for e in ENGS}
        self.dsems = []
        self.n = 0

    def dsem(self, name):
        if name not in self.dma_names:
            self.dma_names[name] = len(self.dma_cnt)
            self.dma_cnt.append(0)
            self.dsems.append(self.pstack.enter_context(self.nc.semaphore(f"ds_{len(self.dsems)}")))
        return self.dma_names[name]

    def _need(self, eng, ticket, waits, raw):
        if ticket is None:
            return
        kind, key, val = ticket
        if kind == "E" and key == eng and (eng == "pe" or not raw):
            return
        k = (kind, key)
        if self.waited[eng].get(k, -1) >= val:
            return
        self.waited[eng][k] = val
        waits.append(ticket)
        if kind == "E":
            o = self.ops[key][val]
            assert not o.get("emitted") or o["sig"], "dependency on already-emitted unsignalled op"
            o["sig"] = True

    def _deps(self, eng, reads, writes):
        waits = []
        for r in reads:
            self._need(eng, r.w, waits, True)
        for r in writes:
            self._need(eng, r.w, waits, False)
            for (kind, key), val in r.rs.items():
                self._need(eng, (kind, key, val), waits, False)
        return waits

    def _commit(self, t, reads, writes):
        k = (t[0], t[1])
        for r in reads:
            if r.rs.get(k, -1) < t[2]:
                r.rs[k] = t[2]
        for r in writes:
            r.w = t
            r.rs = {}

    def op(self, eng, fn, reads=(), writes=()):
        waits = self._deps(eng, reads, writes)
        idx = len(self.ops[eng])
        self.ops[eng].append(dict(fn=fn, waits=waits, sig=False, dma=None))
        t = ("E", eng, idx)
        self._commit(t, reads, writes)
        return t

    def dma(self, q, out_ap, in_ap, reads=(), writes=(), sem="d"):
        waits = self._deps(q, reads, writes)
        sid = self.dsem(sem)
        if self.dma_cnt[sid] > 0:
            self._need(q, ("D", sid, self.dma_cnt[sid]), waits, True)
        self.dma_cnt[sid] += 16
        t = ("D", sid, self.dma_cnt[sid])
        self.ops[q].append(dict(fn=lambda e: e.dma_start(out=out_ap, in_=in_ap), waits=waits, sig=False, dma=sid))
        self._commit(t, reads, writes)
        return t

    def wait_only(self, eng, tickets):
        waits = []
        for t in tickets:
            self._need(eng, t, waits, True)
        self.ops[eng].append(dict(fn=None, waits=waits, sig=False, dma=None))

    def barrier(self):
        tickets = []
        for e in ENGS:
            for idx in range(len(self.ops[e]) - 1, self.done[e] - 1, -1):
                o = self.ops[e][idx]
                if o["fn"] is not None and o["dma"] is None:
                    tickets.append(("E", e, idx))
                    break
        for sid, c in enumerate(self.dma_cnt):
            if c > 0:
                tickets.append(("D", sid, c))
        for e in ENGS:
            self.wait_only(e, tickets)

    def flush(self):
        self.barrier()
        nc = self.nc
        ops = self.ops
        for e in ENGS:
            c = self.cnt[e]
            for o in ops[e][self.done[e]:]:
                if o["sig"]:
                    c += 1
                o["cnt"] = c
                o["emitted"] = True
            self.cnt[e] = c
        esem, dsems = self.esem, self.dsems

        def body(eng, e):
            for o in ops[e][self.done[e]:]:
                for (kind, key, val) in o["waits"]:
                    if kind == "E":
                        eng.wait_ge(esem[key], ops[key][val]["cnt"])
                    else:
                        eng.wait_ge(dsems[key], val)
                if o["fn"] is None:
                    continue
                ins = o["fn"](eng)
                if o["dma"] is not None:
                    ins.then_inc(dsems[o["dma"]], 16)
                elif o["sig"]:
                    ins.then_inc(esem[e], 1)

        with nc.Block() as block:
            for e in ENGS:
                if len(ops[e]) > self.done[e]:
                    getattr(block, BLOCKNAME[e])(lambda eng, e=e: body(eng, e))
        for e in ENGS:
            for o in ops[e][self.done[e]:]:
                o["fn"] = None if o["fn"] is None else True
            self.done[e] = len(ops[e])

    def close(self):
        self.pstack.close()


class Phase:
    def __init__(self, S):
        self.S = S
        self.stack = ExitStack()

    def sb(self, shape, dtype):
        self.S.n += 1
        return self.stack.enter_context(self.S.nc.sbuf_tensor(f"sb{self.S.n}", list(shape), dtype))

    def ps(self, shape=(128, 512), dtype=F32):
        self.S.n += 1
        return self.stack.enter_context(self.S.nc.psum_tensor(f"ps{self.S.n}", list(shape), dtype))

    def end(self):
        self.S.flush()
        self.stack.close()


def build_program(plan):
    nc = bass.Bass("TRN2", target_bir_lowering=False)
    FULL = dict(norm_mix=[4, D], norm_mem=[4, D], norm_ffn=[4, D], w_mem_kv=[4, D, 512], g_mq=[4, 64], g_mk=[4, 64],
                fox_w_in=[2, D, 2572], fox_b_f=[2, 12], fox_g_q=[2, 64], fox_g_k=[2, 64], fox_w_out=[2, D, D],
                conv_w_in=[2, D, 1792], conv_b_in=[2, 1536], conv_dw=[2, 31, 768], conv_dw_b=[2, 768], conv_ln_g=[2, 768],
                conv_ln_b=[2, 768], conv_w_out=[2, D, D], ffn_w_gate=[2, D, DFF], ffn_w_up=[2, D, DFF], ffn_w_down=[2, DFF, D],
                moe_router=[2, D, 8], moe_w_gate=[2, 8, D, EFF], moe_w_up=[2, 8, D, EFF], moe_w_down=[2, 8, EFF, D])
    declared = {}

    class LV:
        def __init__(self, name):
            self.name = name

        def _ap(self, idx, shape, row):
            nm = f"{self.name}_{idx}"
            if nm not in declared:
                declared[nm] = (nc.dram_tensor(nm, list(shape), F32, kind="ExternalInput").ap(), (self.name, idx, row))
            return declared[nm][0]

        def __getitem__(self, key):
            full = FULL[self.name]
            if isinstance(key, int):
                return self._ap(key, full[1:], False)
            if isinstance(key[0], slice):
                return self._ap(key[0].start, [1] + full[1:], True)
            return self._ap(key[0], full[1:], False)[key[1]]

    class LazyDin(dict):
        def __missing__(self, name):
            if name in FULL:
                v = LV(name)
            else:
                shape = dict(hin=[S_TOK, D], mem=[256, D], cst=[128, 512])[name]
                v = nc.dram_tensor(name, shape, F32, kind="ExternalInput").ap()
                declared[name] = (v, None)
            self[name] = v
            return v

    Din = LazyDin()
    out = nc.dram_tensor("out", [S_TOK, D], F32, kind="ExternalOutput").ap()
    hbuf = nc.dram_tensor("hbuf", [S_TOK, D], F32).ap()
    mixbuf = nc.dram_tensor("mixbuf", [8, 128, S_TOK], BF16).ap()
    r_h = regs(NT)
    r_mix = [regs(NTB) for _ in range(8)]

    S = Sched(nc)

    G = Phase(S)
    cst32 = G.sb([128, 512], F32); r_cst = Reg()
    cstb = G.sb([128, 512], BF16)
    epsT = G.sb([128, 1], F32); oneT = G.sb([128, 1], F32); zeroT = G.sb([128, 1], F32)
    onesF = G.sb([128, 128], F32); onesB = G.sb([128, 128], BF16)
    S.dma("sp", cst32[:], Din["cst"][:, :], writes=[r_cst], sem="cst")
    S.op("dve", lambda e: e.tensor_copy(cstb[:], cst32[:]), reads=[r_cst], writes=[r_cst])
    S.op("dve", lambda e: e.memset(epsT[:], EPS), writes=[r_cst])
    S.op("dve", lambda e: e.memset(oneT[:], 1.0), writes=[r_cst])
    S.op("dve", lambda e: e.memset(zeroT[:], 0.0), writes=[r_cst])
    S.op("dve", lambda e: e.memset(onesF[:], 1.0), writes=[r_cst])
    S.op("dve", lambda e: e.memset(onesB[:], 1.0), writes=[r_cst])
    identF = cst32[:, 0:128]; triF = cst32[:, 128:256]; selF = cst32[:, 384:512]
    identB = cstb[:, 0:128]; maskB = cstb[:, 128:256]; blkB = cstb[:, 256:384]
    S.flush()

    def rms_to_T(P, src_ap_fn, ntiles, gain_row_ap, dstT, r_dst_fn, pT, r_pT, tag):
        if not hasattr(P, "rms_tmp"):
            P.rms_tmp = (P.sb([128, D], F32), Reg(), [P.sb([128, D], F32) for _ in range(2)], regs(2),
                         P.sb([128, D], BF16), Reg(), [P.sb([128, 1], F32) for _ in range(2)], regs(2),
                         [P.sb([128, 1], F32) for _ in range(2)], regs(2), [P.sb([128, D], BF16) for _ in range(2)], regs(2))
        gbc, r_g, hl, r_hl, junk, r_junk, ss, r_ss, sd, r_sd, ub, r_ub = P.rms_tmp
        S.dma("sp", gbc[:], gain_row_ap.partition_broadcast(128), writes=[r_g], sem="rmsg")
        for t in range(ntiles):
            b = t % 2
            src, rsrc = src_ap_fn(t)
            S.dma("sp", hl[b][:], src, reads=rsrc, writes=[r_hl[b]], sem=f"{tag}h{b}")
            S.op("act", lambda e, b=b: e.activation(junk[:], hl[b][:], AF.Square, accum_out=ss[b][:, 0:1]),
                 reads=[r_hl[b]], writes=[r_junk, r_ss[b]])
            S.op("act", lambda e, b=b: e.activation(sd[b][:], ss[b][:], AF.Sqrt, bias=epsT[:, 0:1], scale=1.0 / D),
                 reads=[r_ss[b]], writes=[r_sd[b]])
            S.op("dve", lambda e, b=b: e.reciprocal(sd[b][:], sd[b][:]), reads=[r_sd[b]], writes=[r_sd[b]])
            S.op("dve", lambda e, b=b: e.scalar_tensor_tensor(ub[b][:], hl[b][:], sd[b][:, 0:1], gbc[:], ALU.mult, ALU.mult),
                 reads=[r_hl[b], r_sd[b], r_g], writes=[r_ub[b]])
            pt = pT[b]
            for k in range(8):
                S.op("pe", lambda e, b=b, k=k, pt=pt: e.transpose(pt[:, k * 128:(k + 1) * 128], ub[b][:, k * 128:(k + 1) * 128], identB),
                     reads=[r_ub[b], r_cst], writes=[r_pT[b]])
            eng = "act" if t % 2 == 0 else "dve"
            dst = dstT[:, :, t * 128:(t + 1) * 128]
            srcv = pt[:, :].rearrange("p (k n) -> p k n", k=8)
            if eng == "act":
                S.op("act", lambda e, dst=dst, srcv=srcv: e.copy(dst, srcv), reads=[r_pT[b]], writes=[r_dst_fn(t)])
            else:
                S.op("dve", lambda e, dst=dst, srcv=srcv: e.tensor_copy(dst, srcv), reads=[r_pT[b]], writes=[r_dst_fn(t)])

    def fm_norm(P, tmp, psrc, r_psrc, gcol, r_gcol, dst, r_dst, N, pS, r_pS, idx):
        sqb, r_sq, rs, r_rs = tmp
        b = idx % 2
        S.op("act", lambda e: e.activation(sqb[b][:, :N], psrc, AF.Square), reads=[r_psrc], writes=[r_sq[b]])
        S.op("pe", lambda e: e.matmul(pS[:, :N], blkB, sqb[b][:, :N], start=True, stop=True), reads=[r_sq[b], r_cst], writes=[r_pS])
        S.op("act", lambda e: e.activation(rs[b][:, :N], pS[:, :N], AF.Sqrt, bias=epsT[:, 0:1], scale=1.0 / 64),
             reads=[r_pS], writes=[r_rs[b]])
        S.op("dve", lambda e: e.reciprocal(rs[b][:, :N], rs[b][:, :N]), reads=[r_rs[b]], writes=[r_rs[b]])
        S.op("dve", lambda e: e.scalar_tensor_tensor(dst, psrc, gcol, rs[b][:, :N], ALU.mult, ALU.mult),
             reads=[r_psrc, r_rs[b], r_gcol], writes=[r_dst])

    def fm_tmp(P):
        return ([P.sb([128, 512], BF16) for _ in range(2)], regs(2), [P.sb([128, 512], F32) for _ in range(2)], regs(2))

    def load_gvec(P, rows, tag):
        n = len(rows)
        gv = P.sb([n, 128], F32); r_gv = Reg()
        for i, r in enumerate(rows):
            for hf in range(2):
                S.dma("sp", gv[i:i + 1, hf * 64:(hf + 1) * 64], r, writes=[r_gv], sem=tag)
        return gv, r_gv

    def mixer_phase(i, hin):
        j = i // 2
        fox = (i % 2 == 0)
        PO = Phase(S)
        if not fox:
            vt = PO.sb([36, 768], F32); r_vt = Reg()
            vT = PO.sb([128, 6, 36], F32); r_vT = Reg()
            ugT = PO.sb([128, 6, S_TOK + 32], BF16); r_ug = [regs(NTB) for _ in range(6)]
        P = Phase(S)
        uT = P.sb([128, 8, S_TOK], BF16); r_uT = regs(NTB)
        pA = [P.ps() for _ in range(2)]; r_pA = regs(2)
        pS = P.ps(); r_pS = Reg()
        pST = [P.ps() for _ in range(2)]; r_pST = regs(2)
        pO = [P.ps() for _ in range(2)]; r_pO = regs(2)
        pX = P.ps(); r_pX = Reg()
        pXb = pX[:, :].bitcast(BF16)
        pTb = [pXb, pO[0][:, :].bitcast(BF16)]
        rms_to_T(P, lambda t: (hin[t * 128:(t + 1) * 128, :], [r_h[t]]), NT, Din["norm_mix"][i:i + 1, :],
                 uT, lambda t: r_uT[t // 4], pTb, [r_pX, r_pO[0]], f"u{i}")
        tmp = fm_tmp(P)
        rows = ([Din["fox_g_q"][j:j + 1, :], Din["fox_g_k"][j:j + 1, :]] if fox else []) + \
               [Din["g_mq"][i:i + 1, :], Din["g_mk"][i:i + 1, :]]
        gv, r_gv = load_gvec(P, rows, f"gv{i}")
        gT = P.sb([128, 4], F32); r_gT = Reg()
        ng = len(rows)
        S.op("pe", lambda e: e.transpose(pS[:, 0:ng], gv[0:ng, :], identF[0:ng, 0:ng]), reads=[r_gv, r_cst], writes=[r_pS])
        S.op("dve", lambda e: e.tensor_copy(gT[:, 0:ng], pS[:, 0:ng]), reads=[r_pS], writes=[r_gT])
        c_gq, c_gk = (0, 1) if fox else (None, None)
        c_gmq, c_gmk = (2, 3) if fox else (0, 1)

        memT = P.sb([128, 8, 256], BF16); r_memT = Reg()
        rms_to_T(P, lambda t: (Din["mem"][t * 128:(t + 1) * 128, :], []), 2, Din["norm_mem"][i:i + 1, :],
                 memT, lambda t: r_memT, pTb, [r_pX, r_pO[0]], f"m{i}")
        wkv = P.sb([128, 8, 512], BF16); r_wkv = Reg()
        S.dma("pool", wkv[:], Din["w_mem_kv"][i].rearrange("(k p) n -> p k n", p=128), writes=[r_wkv], sem=f"wkv")
        kmT = P.sb([128, 2, 256], BF16); r_kmT = Reg()
        V1m = P.sb([128, 2, 4, 65], BF16); r_V1m = Reg()
        S.op("dve", lambda e: e.memset(V1m[:], 1.0), writes=[r_V1m])
        cnt = [0]

        def nxtA():
            cnt[0] += 1
            return pA[cnt[0] % 2], r_pA[cnt[0] % 2], cnt[0]

        for pr in range(2):
            p, rp, idx = nxtA()
            for k in range(8):
                S.op("pe", lambda e, p=p, k=k, pr=pr: e.matmul(p[:, 0:256], wkv[:, k, pr * 128:(pr + 1) * 128], memT[:, k, :], start=(k == 0), stop=(k == 7)),
                     reads=[r_wkv, r_memT], writes=[rp])
            fm_norm(P, tmp, p[:, 0:256], rp, gT[:, c_gmk:c_gmk + 1], r_gT, kmT[:, pr, :], r_kmT, 256, pS, r_pS, idx)
        for mt in range(2):
            p, rp, idx = nxtA()
            for k in range(8):
                S.op("pe", lambda e, p=p, k=k, mt=mt: e.matmul(p[:, 0:256], memT[:, k, mt * 128:(mt + 1) * 128], wkv[:, k, 256:512], start=(k == 0), stop=(k == 7)),
                     reads=[r_wkv, r_memT], writes=[rp])
            S.op("act", lambda e, p=p, mt=mt: e.copy(V1m[:, mt, :, 0:64], p[:, 0:256].rearrange("p (h d) -> p h d", h=4)),
                 reads=[rp], writes=[r_V1m])

        qT = P.sb([128, S_TOK], BF16); r_qT = regs(NTB)
        Pt = [P.sb([128, 512], BF16) for _ in range(3)]; r_Pt = regs(3)
        Osb = [P.sb([128, 512], F32) for _ in range(2)]; r_Osb = regs(2)
        rden = [P.sb([64, 512], F32) for _ in range(2)]; r_rden = regs(2)
        mixo = [P.sb([128, 512], BF16) for _ in range(2)]; r_mixo = regs(2)
        st = dict(p=0, o=0, m=0)

        def in_proj_fm(w, r_w, dstT, r_dstT, gcol):
            for tb in range(NTB):
                p, rp, idx = nxtA()
                for k in range(8):
                    S.op("pe", lambda e, p=p, k=k, tb=tb: e.matmul(p[:, :], w[:, k, :], uT[:, k, tb * 512:(tb + 1) * 512], start=(k == 0), stop=(k == 7)),
                         reads=[r_w, r_uT[tb]], writes=[rp])
                fm_norm(P, tmp, p[:, :], rp, gcol, r_gT, dstT[:, tb * 512:(tb + 1) * 512], r_dstT[tb], 512, pS, r_pS, idx)

        def finish_head(po, r_po, hp, qb, chunk, last):
            o = st["o"] % 2; st["o"] += 1
            m = st["m"] % 2
            S.op("dve", lambda e: e.tensor_copy(Osb[o][0:65, :], po[0:65, :]), reads=[r_po], writes=[r_Osb[o]])
            S.op("pe", lambda e: e.matmul(pS[0:64, :], selF[0:65, 0:64], Osb[o][0:65, :], start=True, stop=True),
                 reads=[r_Osb[o], r_cst], writes=[r_pS])
            S.op("dve", lambda e: e.reciprocal(rden[o][:, :], pS[0:64, :]), reads=[r_pS], writes=[r_rden[o]])
            S.op("dve", lambda e: e.tensor_tensor(mixo[m][hp * 64:(hp + 1) * 64, :], Osb[o][0:64, :], rden[o][:, :], ALU.mult),
                 reads=[r_Osb[o], r_rden[o]], writes=[r_mixo[m]])
            if last:
                S.dma("sp", mixbuf[chunk, :, qb * 512:(qb + 1) * 512], mixo[m][:, :], reads=[r_mixo[m]], writes=[r_mix[chunk][qb]], sem=f"mixo{m}")
                st["m"] += 1

        def mem_attention(mqT, r_mqT, pr):
            tasks = [(qb, hp, mt) for qb in range(NTB) for hp in range(2) for mt in range(2)]
            slots = {}

            def emit_qk(ti):
                qb, hp, mt = tasks[ti]
                pi = st["p"]; st["p"] += 1
                slots[ti] = pi
                ps_, rps = pST[pi % 2], r_pST[pi % 2]
                S.op("pe", lambda e, ps_=ps_, mt=mt, hp=hp, qb=qb: e.matmul(ps_[:, :], kmT[hp * 64:(hp + 1) * 64, pr, mt * 128:(mt + 1) * 128],
                                                                          mqT[hp * 64:(hp + 1) * 64, qb * 512:(qb + 1) * 512], start=True, stop=True),
                     reads=[r_kmT, r_mqT[qb]], writes=[rps])

            emit_qk(0)
            cur = {}
            for ti, (qb, hp, mt) in enumerate(tasks):
                if ti + 1 < len(tasks):
                    emit_qk(ti + 1)
                if mt == 0:
                    cur["po"], cur["r_po"] = pO[st["o"] % 2], r_pO[st["o"] % 2]
                po, r_po = cur["po"], cur["r_po"]
                pslot = slots.pop(ti)
                ps_, rps = pST[pslot % 2], r_pST[pslot % 2]
                pi = pslot % 3
                S.op("act", lambda e, ps_=ps_, pi=pi: e.activation(Pt[pi][:, :], ps_[:, :], AF.Exp, bias=zeroT[:, 0:1], scale=0.125),
                     reads=[rps], writes=[r_Pt[pi]])
                S.op("pe", lambda e, po=po, pi=pi, mt=mt, hp=hp: e.matmul(po[0:65, :], V1m[:, mt, 2 * pr + hp, :], Pt[pi][:, :], start=(mt == 0), stop=(mt == 1)),
                     reads=[r_V1m, r_Pt[pi]], writes=[r_po])
                if mt == 1:
                    finish_head(po, r_po, hp, qb, 6 + pr, hp == 1)

        Win = Din["fox_w_in"][j] if fox else Din["conv_w_in"][j]
        wq = P.sb([128, 8, 128], BF16); r_wq = Reg()
        wk = P.sb([128, 8, 128], BF16); r_wk = Reg()
        wv = P.sb([128, 8, 128], BF16); r_wv = Reg()

        def load_w(dst, r_dst, col0, ncol, sem):
            S.dma("pool", dst[:, :, 0:ncol], Win[:, col0:col0 + ncol].rearrange("(k p) n -> p k n", p=128), writes=[r_dst], sem=sem)

        if fox:
            kT = P.sb([128, S_TOK], BF16); r_kT = regs(NTB)
            V1 = P.sb([128, NT, 2, 65], BF16); r_V1 = regs(NTB)
            S.op("dve", lambda e: e.memset(V1[:], 1.0), writes=r_V1)
            wf = P.sb([128, 8, 12], BF16); r_wf = Reg()
            load_w(wf, r_wf, 2304, 12, "wf")
            bfb = P.sb([128, 12], F32); r_bfb = Reg()
            S.dma("sp", bfb[:], Din["fox_b_f"][j:j + 1, :].partition_broadcast(128), writes=[r_bfb], sem="bfb")
            Ctok = P.sb([128, 12, NT], F32); r_C = Reg()
            carry = P.sb([128, 12, NT + 1], F32); r_carry = Reg()
            S.op("dve", lambda e: e.memset(carry[:], 0.0), writes=[r_carry])
            nl = [P.sb([128, 12], F32) for _ in range(2)]; r_nl = regs(2)
            tl = [P.sb([128, 12], F32) for _ in range(2)]; r_tl = regs(2)
            for t in range(NT):
                b = t % 2
                p, rp, idx = nxtA()
                for k in range(8):
                    S.op("pe", lambda e, p=p, k=k, t=t: e.matmul(p[:, 0:12], uT[:, k, t * 128:(t + 1) * 128], wf[:, k, :], start=(k == 0), stop=(k == 7)),
                         reads=[r_wf, r_uT[t // 4]], writes=[rp])
                S.op("dve", lambda e, p=p, b=b: e.tensor_tensor(tl[b][:], p[:, 0:12], bfb[:], ALU.add), reads=[rp, r_bfb], writes=[r_tl[b]])
                S.op("act", lambda e, b=b: e.activation(tl[b][:], tl[b][:], AF.Exp, bias=zeroT[:, 0:1], scale=-1.0), reads=[r_tl[b]], writes=[r_tl[b]])
                S.op("act", lambda e, b=b: e.activation(nl[b][:], tl[b][:], AF.Ln, bias=oneT[:, 0:1], scale=1.0), reads=[r_tl[b]], writes=[r_nl[b]])
                S.op("pe", lambda e, b=b: e.matmul(pS[:, 0:12], triF, nl[b][:], start=True, stop=True), reads=[r_nl[b], r_cst], writes=[r_pS])
                S.op("pe", lambda e, b=b: e.matmul(pS[:, 16:28], onesF[:, :], nl[b][:], start=True, stop=True), reads=[r_nl[b], r_cst], writes=[r_pS])
                S.op("dve", lambda e, t=t: e.tensor_tensor(Ctok[:, :, t], pS[:, 0:12], carry[:, :, t], ALU.add), reads=[r_pS, r_carry], writes=[r_C])
                S.op("dve", lambda e, t=t: e.tensor_tensor(carry[:, :, t + 1], pS[:, 16:28], carry[:, :, t], ALU.add), reads=[r_pS, r_carry], writes=[r_carry])
            Bq = [P.sb([128, 4], F32) for _ in range(2)]; r_Bq = regs(2)
            nb = [0]
            for pj in range(6):
                load_w(wq, r_wq, pj * 128, 128, "wq")
                load_w(wk, r_wk, 768 + pj * 128, 128, "wk")
                load_w(wv, r_wv, 1536 + pj * 128, 128, "wv")
                in_proj_fm(wq, r_wq, qT, r_qT, gT[:, c_gq:c_gq + 1])
                in_proj_fm(wk, r_wk, kT, r_kT, gT[:, c_gk:c_gk + 1])
                for t in range(NT):
                    p, rp, idx = nxtA()
                    for k in range(8):
                        S.op("pe", lambda e, p=p, k=k, t=t: e.matmul(p[:, 0:128], uT[:, k, t * 128:(t + 1) * 128], wv[:, k, :], start=(k == 0), stop=(k == 7)),
                             reads=[r_wv, r_uT[t // 4]], writes=[rp])
                    ev = "act" if t % 2 == 0 else "dve"
                    dst = V1[:, t, :, 0:64]
                    srcv = p[:, 0:128].rearrange("p (h d) -> p h d", h=2)
                    if ev == "act":
                        S.op("act", lambda e, dst=dst, srcv=srcv: e.copy(dst, srcv), reads=[rp], writes=[r_V1[t // 4]])
                    else:
                        S.op("dve", lambda e, dst=dst, srcv=srcv: e.tensor_copy(dst, srcv), reads=[rp], writes=[r_V1[t // 4]])
                tasks = [(qb, hp, kt, 4 * qb + 4) for qb in range(NTB) for hp in range(2) for kt in range(4 * qb + 4)]
                slots = {}

                def emit_qk(ti):
                    qb, hp, kt, nk = tasks[ti]
                    pi = st["p"]; st["p"] += 1
                    slots[ti] = pi
                    ps_, rps = pST[pi % 2], r_pST[pi % 2]
                    S.op("pe", lambda e, ps_=ps_, kt=kt, hp=hp, qb=qb: e.matmul(ps_[:, :], kT[hp * 64:(hp + 1) * 64, kt * 128:(kt + 1) * 128],
                                                                              qT[hp * 64:(hp + 1) * 64, qb * 512:(qb + 1) * 512], start=True, stop=True),
                         reads=[r_kT[kt // 4], r_qT[qb]], writes=[rps])

                emit_qk(0)
                cur = {}
                for ti, (qb, hp, kt, nk) in enumerate(tasks):
                    if ti + 1 < len(tasks):
                        emit_qk(ti + 1)
                    h = 2 * pj + hp
                    if kt == 0:
                        cur["po"], cur["r_po"] = pO[st["o"] % 2], r_pO[st["o"] % 2]
                    po, r_po = cur["po"], cur["r_po"]
                    pslot = slots.pop(ti)
                    ps_, rps = pST[pslot % 2], r_pST[pslot % 2]
                    pi = pslot % 3
                    bq = nb[0] % 2; nb[0] += 1
                    S.op("dve", lambda e, bq=bq, h=h, qb=qb, kt=kt: e.tensor_scalar(Bq[bq][:, :], carry[:, h, 4 * qb:4 * qb + 4], Ctok[:, h, kt:kt + 1], -1.0, ALU.subtract, ALU.mult),
                         reads=[r_carry, r_C], writes=[r_Bq[bq]])
                    r = kt - 4 * qb
                    for jj in range(4):
                        if r > jj:
                            continue
                        S.op("act", lambda e, ps_=ps_, pi=pi, jj=jj, bq=bq: e.activation(Pt[pi][:, jj * 128:(jj + 1) * 128], ps_[:, jj * 128:(jj + 1) * 128], AF.Exp,
                                                                                        bias=Bq[bq][:, jj:jj + 1], scale=0.125),
                             reads=[rps, r_Bq[bq]], writes=[r_Pt[pi]])
                    if r >= 0:
                        S.op("dve", lambda e, pi=pi, r=r: e.tensor_tensor(Pt[pi][:, r * 128:(r + 1) * 128], Pt[pi][:, r * 128:(r + 1) * 128], maskB, ALU.mult),
                             reads=[r_Pt[pi], r_cst], writes=[r_Pt[pi]])
                        if r > 0:
                            S.op("dve", lambda e, pi=pi, r=r: e.memset(Pt[pi][:, 0:r * 128], 0.0), writes=[r_Pt[pi]])
                    S.op("pe", lambda e, po=po, pi=pi, kt=kt, hp=hp, nk=nk: e.matmul(po[0:65, :], V1[:, kt, hp, :], Pt[pi][:, :], start=(kt == 0), stop=(kt == nk - 1)),
                         reads=[r_V1[kt // 4], r_Pt[pi]], writes=[r_po])
                    if kt == nk - 1:
                        finish_head(po, r_po, hp, qb, pj, hp == 1)
            for pr in range(2):
                load_w(wq, r_wq, 2316 + pr * 128, 128, "wq")
                in_proj_fm(wq, r_wq, qT, r_qT, gT[:, c_gmq:c_gmq + 1])
                mem_attention(qT, r_qT, pr)
        else:
            S.dma("sp", vt[0:31, :], Din["conv_dw"][j], writes=[r_vt], sem="vt")
            S.dma("sp", vt[31:32, :], Din["conv_dw_b"][j:j + 1, :], writes=[r_vt], sem="vt")
            S.dma("sp", vt[32:33, :], Din["conv_ln_g"][j:j + 1, :], writes=[r_vt], sem="vt")
            S.dma("sp", vt[33:34, :], Din["conv_ln_b"][j:j + 1, :], writes=[r_vt], sem="vt")
            S.dma("sp", vt[34:36, :], Din["conv_b_in"][j].rearrange("(a n) -> a n", a=2), writes=[r_vt], sem="vt")
            for c in range(6):
                S.op("pe", lambda e, c=c: e.transpose(pS[:, 0:36], vt[0:36, c * 128:(c + 1) * 128], identF[0:36, 0:36]), reads=[r_vt, r_cst], writes=[r_pS])
                S.op("dve", lambda e, c=c: e.tensor_copy(vT[:, c, :], pS[:, 0:36]), reads=[r_pS], writes=[r_vT])
            S.op("dve", lambda e: e.memset(ugT[:, :, 0:32], 0.0), writes=[r_ug[c][0] for c in range(6)])
            sgm = [P.sb([128, 512], F32) for _ in range(2)]; r_sgm = regs(2)
            OFF = 32
            for c in range(6):
                load_w(wq, r_wq, c * 128, 128, "wq")
                load_w(wk, r_wk, 768 + c * 128, 128, "wk")
                for tb in range(NTB):
                    pa, rpa, _ = nxtA()
                    pb, rpb, idx = nxtA()
                    for k in range(8):
                        S.op("pe", lambda e, pa=pa, k=k, tb=tb: e.matmul(pa[:, :], wq[:, k, :], uT[:, k, tb * 512:(tb + 1) * 512], start=(k == 0), stop=(k == 7)),
                             reads=[r_wq, r_uT[tb]], writes=[rpa])
                    for k in range(8):
                        S.op("pe", lambda e, pb=pb, k=k, tb=tb: e.matmul(pb[:, :], wk[:, k, :], uT[:, k, tb * 512:(tb + 1) * 512], start=(k == 0), stop=(k == 7)),
                             reads=[r_wk, r_uT[tb]], writes=[rpb])
                    b = idx % 2
                    S.op("act", lambda e, pb=pb, b=b, c=c: e.activation(sgm[b][:, :], pb[:, :], AF.Sigmoid, bias=vT[:, c, 35:36], scale=1.0),
                         reads=[rpb, r_vT], writes=[r_sgm[b]])
                    S.op("dve", lambda e, pa=pa, b=b, c=c, tb=tb: e.scalar_tensor_tensor(ugT[:, c, OFF + tb * 512:OFF + (tb + 1) * 512], pa[:, :], vT[:, c, 34:35], sgm[b][:, :], ALU.add, ALU.mult),
                         reads=[rpa, r_sgm[b], r_vT], writes=[r_ug[c][tb]])
            for pr in range(2):
                load_w(wv, r_wv, 1536 + pr * 128, 128, "wv")
                in_proj_fm(wv, r_wv, qT, r_qT, gT[:, c_gmq:c_gmq + 1])
                mem_attention(qT, r_qT, pr)
            P.end()
            P = Phase(S)
            xc = [P.sb([128, 6, 512], F32) for _ in range(2)]; r_xc = [regs(6) for _ in range(2)]
            sq = P.sb([128, 512], F32); r_sqx = Reg()
            mean = P.sb([128, 512], F32); r_mean = Reg()
            rstd = P.sb([128, 512], F32); r_rstd = Reg()
            t1 = [P.sb([128, 512], F32) for _ in range(2)]; r_t1 = regs(2)
            mixo = [P.sb([128, 512], BF16) for _ in range(2)]; r_mixo = regs(2)
            pM, r_pM = P.ps(), Reg()
            pV, r_pV = P.ps(), Reg()
            for tb in range(NTB):
                xb = tb % 2
                for c in range(6):
                    acc = xc[xb][:, c, :]
                    rd = [r_ug[c][tb]] + ([r_ug[c][tb - 1]] if tb > 0 else []) + [r_vT]
                    base = OFF + tb * 512 - 30
                    S.op("dve", lambda e, acc=acc, c=c, base=base: e.tensor_scalar(acc, ugT[:, c, base:base + 512], vT[:, c, 0:1], vT[:, c, 31:32], ALU.mult, ALU.add),
                         reads=rd, writes=[r_xc[xb][c]])
                    for w in range(1, 31):
                        S.op("dve", lambda e, acc=acc, c=c, base=base, w=w: e.scalar_tensor_tensor(acc, ugT[:, c, base + w:base + w + 512], vT[:, c, w:w + 1], acc, ALU.mult, ALU.add),
                             reads=rd, writes=[r_xc[xb][c]])
                for c in range(6):
                    S.op("pe", lambda e, c=c, xb=xb: e.matmul(pM[:, :], onesF[:, :], xc[xb][:, c, :], start=(c == 0), stop=(c == 5)),
                         reads=[r_xc[xb][c], r_cst], writes=[r_pM])
                for c in range(6):
                    S.op("act", lambda e, c=c, xb=xb: e.activation(sq[:, :], xc[xb][:, c, :], AF.Square), reads=[r_xc[xb][c]], writes=[r_sqx])
                    S.op("pe", lambda e, c=c: e.matmul(pV[:, :], onesF[:, :], sq[:, :], start=(c == 0), stop=(c == 5)),
                         reads=[r_sqx, r_cst], writes=[r_pV])
                S.op("dve", lambda e: e.tensor_scalar(mean[:, :], pM[:, :], 1.0 / 768, None, ALU.mult), reads=[r_pM], writes=[r_mean])
                S.op("dve", lambda e: e.tensor_tensor(rstd[:, :], mean[:, :], mean[:, :], ALU.mult), reads=[r_mean], writes=[r_rstd])
                S.op("dve", lambda e: e.scalar_tensor_tensor(rstd[:, :], pV[:, :], 1.0 / 768, rstd[:, :], ALU.mult, ALU.subtract), reads=[r_pV, r_rstd], writes=[r_rstd])
                S.op("act", lambda e: e.activation(rstd[:, :], rstd[:, :], AF.Sqrt, bias=epsT[:, 0:1], scale=1.0), reads=[r_rstd], writes=[r_rstd])
                S.op("dve", lambda e: e.reciprocal(rstd[:, :], rstd[:, :]), reads=[r_rstd], writes=[r_rstd])
                for c in range(6):
                    tt = c % 2
                    m = st["m"] % 2; st["m"] += 1
                    S.op("dve", lambda e, c=c, xb=xb, tt=tt: e.tensor_tensor(t1[tt][:, :], xc[xb][:, c, :], mean[:, :], ALU.subtract), reads=[r_xc[xb][c], r_mean], writes=[r_t1[tt]])
                    S.op("dve", lambda e, tt=tt: e.tensor_tensor(t1[tt][:, :], t1[tt][:, :], rstd[:, :], ALU.mult), reads=[r_t1[tt], r_rstd], writes=[r_t1[tt]])
                    S.op("act", lambda e, c=c, tt=tt, m=m: e.activation(mixo[m][:, :], t1[tt][:, :], AF.Silu, bias=vT[:, c, 33:34], scale=vT[:, c, 32:33]),
                         reads=[r_t1[tt], r_vT], writes=[r_mixo[m]])
                    S.dma("sp", mixbuf[c, :, tb * 512:(tb + 1) * 512], mixo[m][:, :], reads=[r_mixo[m]], writes=[r_mix[c][tb]], sem=f"mixo{m}")
        P.end()
        PO.stack.close()

    def outproj_phase(i, hin, hout):
        j = i // 2
        Wo = Din["fox_w_out"][j] if i % 2 == 0 else Din["conv_w_out"][j]
        P = Phase(S)
        wout = P.sb([128, 8, D], BF16); r_wo = Reg()
        S.dma("pool", wout[:], Wo.rearrange("(k p) n -> p k n", p=128), writes=[r_wo], sem="wout")
        mixl = [P.sb([128, 8, 512], BF16) for _ in range(2)]; r_ml = regs(2)
        hl = [P.sb([128, D], F32) for _ in range(3)]; r_hl = regs(3)
        pd = [[P.ps() for _ in range(2)] for _ in range(2)]; r_pd = [regs(2) for _ in range(2)]
        for tb in range(NTB):
            mb = tb % 2
            S.dma("sp", mixl[mb][:], mixbuf[:, :, tb * 512:(tb + 1) * 512].rearrange("c p n -> p c n"),
                  reads=[r_mix[c][tb] for c in range(8)], writes=[r_ml[mb]], sem=f"ml{mb}")
            for tt in range(4):
                t = tb * 4 + tt
                hb = t % 3
                pb = t % 2
                S.dma("sp", hl[hb][:], hin[t * 128:(t + 1) * 128, :], reads=[r_h[t]], writes=[r_hl[hb]], sem=f"oh{hb}")
                for half in range(2):
                    for c in range(8):
                        S.op("pe", lambda e, pb=pb, half=half, c=c, mb=mb, tt=tt: e.matmul(pd[pb][half][:, :], mixl[mb][:, c, tt * 128:(tt + 1) * 128], wout[:, c, half * 512:(half + 1) * 512],
                                                                                          start=(c == 0), stop=(c == 7)),
                             reads=[r_ml[mb], r_wo], writes=[r_pd[pb][half]])
                    S.op("dve", lambda e, pb=pb, half=half, hb=hb: e.tensor_tensor(hl[hb][:, half * 512:(half + 1) * 512], pd[pb][half][:, :], hl[hb][:, half * 512:(half + 1) * 512], ALU.add),
                         reads=[r_pd[pb][half], r_hl[hb]], writes=[r_hl[hb]])
                S.dma("sp", hout[t * 128:(t + 1) * 128, :], hl[hb][:], reads=[r_hl[hb]], writes=[r_h[t]], sem=f"os{hb}")
        P.end()

    def ffn_phase(i, hin, hout, sbks=(0, 1)):
        j = i // 2
        moe = (i % 2 == 1)
        NE = NEXP if moe else 1
        FF = EFF if moe else DFF
        GC = 4 if moe else 2
        NCG = FF // 128 // GC
        NSB = 2
        TSB = S_TOK // NSB
        NTS = TSB // 128
        for sbk in sbks:
            P = Phase(S)
            acc = P.sb([128, NTS, D], F32); r_acc = regs(NTS)
            zT = P.sb([128, 8, TSB], BF16); r_zT = regs(TSB // 256)
            gates = P.sb([128, NTS, 8], F32); r_gates = regs(NTS)
            gbc = P.sb([128, D], F32); r_g = Reg()
            S.dma("sp", gbc[:], Din["norm_ffn"][i:i + 1, :].partition_broadcast(128), writes=[r_g], sem="fg")
            wg = [P.sb([128, 8, GC * 128], BF16) for _ in range(2)]; r_wg = regs(2)
            wu = [P.sb([128, 8, GC * 128], BF16) for _ in range(2)]; r_wu = regs(2)
            wd = [P.sb([128, GC, D], BF16) for _ in range(2)]; r_wd = regs(2)
            aT = [P.sb([128, GC, 256], BF16) for _ in range(2)]; r_aT = regs(2)
            sg = [P.sb([128, 256], F32) for _ in range(3)]; r_sg = regs(3)
            pd = [[P.ps() for _ in range(2)] for _ in range(2)]; r_pd = [regs(2) for _ in range(2)]
            pg = [P.ps() for _ in range(2)]; r_pg = regs(2)
            pu = [P.ps() for _ in range(2)]; r_pu = regs(2)
            junk = P.sb([128, D], BF16); r_junk = Reg()
            ss = [P.sb([128, 1], F32) for _ in range(2)]; r_ss = regs(2)
            sd = [P.sb([128, 1], F32) for _ in range(2)]; r_sd = regs(2)
            z32 = [P.sb([128, D], F32) for _ in range(2)]; r_z32 = regs(2)
            if moe:
                wr = P.sb([128, 8, 8], F32); r_wr = Reg()
                wrA = P.sb([8, 1024], F32); r_wrA = Reg()
                S.dma("sp", wrA[:], Din["moe_router"][j].rearrange("(i r) n -> i (r n)", i=8), writes=[r_wrA], sem="wr")
                wrAv = wrA[0:8, :].rearrange("i (r n) -> i r n", n=8)
                for n in range(8):
                    S.op("pe", lambda e, n=n: e.transpose(pg[0][:, n * 8:(n + 1) * 8], wrAv[:, :, n], identF[0:8, 0:8]), reads=[r_wrA, r_cst], writes=[r_pg[0]])
                S.op("act", lambda e: e.copy(wr[:, :, :].rearrange("p k n -> p n k"), pg[0][:, 0:64].rearrange("p (n k) -> p n k", n=8)), reads=[r_pg[0]], writes=[r_wr])
                zT32 = [P.sb([128, 8, 128], F32) for _ in range(2)]; r_zT32 = regs(2)
                rt = {n: [P.sb([128, 8], F32) for _ in range(2)] for n in ("L", "eq", "L2", "sel", "ex")}
                rc = {n: [P.sb([128, 1], F32) for _ in range(2)] for n in ("m1", "m2", "nm1", "den")}
                r_rt = regs(2)
            for t in range(NTS):
                b = t % 2
                tg = sbk * NTS + t
                S.dma("sp", acc[:, t, :], hin[tg * 128:(tg + 1) * 128, :], reads=[r_h[tg]], writes=[r_acc[t]], sem=f"fh{b}")
                S.op("act", lambda e, b=b, t=t: e.activation(junk[:], acc[:, t, :], AF.Square, accum_out=ss[b][:, 0:1]),
                     reads=[r_acc[t]], writes=[r_junk, r_ss[b]])
                S.op("act", lambda e, b=b: e.activation(sd[b][:], ss[b][:], AF.Sqrt, bias=epsT[:, 0:1], scale=1.0 / D), reads=[r_ss[b]], writes=[r_sd[b]])
                S.op("dve", lambda e, b=b: e.reciprocal(sd[b][:], sd[b][:]), reads=[r_sd[b]], writes=[r_sd[b]])
                S.op("dve", lambda e, b=b, t=t: e.scalar_tensor_tensor(z32[b][:], acc[:, t, :], sd[b][:, 0:1], gbc[:], ALU.mult, ALU.mult),
                     reads=[r_acc[t], r_sd[b], r_g], writes=[r_z32[b]])
                for hf in range(2):
                    pt, rpt = pg[hf], r_pg[hf]
                    for kk in range(4):
                        k = hf * 4 + kk
                        S.op("pe", lambda e, pt=pt, kk=kk, k=k, b=b: e.transpose(pt[:, kk * 128:(kk + 1) * 128], z32[b][:, k * 128:(k + 1) * 128], identF),
                             reads=[r_z32[b], r_cst], writes=[rpt])
                    S.op("act", lambda e, pt=pt, hf=hf, t=t: e.copy(zT[:, hf * 4:(hf + 1) * 4, t * 128:(t + 1) * 128], pt[:, :].rearrange("p (k n) -> p k n", k=4)),
                         reads=[rpt], writes=[r_zT[t // 2]])
                    if moe:
                        S.op("act", lambda e, pt=pt, hf=hf, b=b: e.copy(zT32[b][:, hf * 4:(hf + 1) * 4, :], pt[:, :].rearrange("p (k n) -> p k n", k=4)),
                             reads=[rpt], writes=[r_zT32[b]])
                if moe:
                    pl, rpl = pu[b], r_pu[b]
                    for k in range(8):
                        S.op("pe", lambda e, pl=pl, k=k, b=b: e.matmul(pl[:, 0:8], zT32[b][:, k, :], wr[:, k, :], start=(k == 0), stop=(k == 7)),
                             reads=[r_zT32[b], r_wr], writes=[rpl])
                    L, eq, L2, sel, ex = (rt[n][b] for n in ("L", "eq", "L2", "sel", "ex"))
                    m1, m2, nm1, den = (rc[n][b] for n in ("m1", "m2", "nm1", "den"))
                    R = [r_rt[b]]
                    S.op("dve", lambda e, L=L, pl=pl: e.tensor_copy(L[:], pl[:, 0:8]), reads=[rpl], writes=R)
                    S.op("dve", lambda e, L=L, m1=m1: e.tensor_reduce(m1[:], L[:], AX.X, ALU.max), reads=R, writes=R)
                    S.op("dve", lambda e, L=L, m1=m1, eq=eq: e.tensor_scalar(eq[:], L[:], m1[:, 0:1], None, ALU.is_ge), reads=R, writes=R)
                    S.op("dve", lambda e, L=L, L2=L2, eq=eq: e.scalar_tensor_tensor(L2[:], eq[:], -1e30, L[:], ALU.mult, ALU.add), reads=R, writes=R)
                    S.op("dve", lambda e, L2=L2, m2=m2: e.tensor_reduce(m2[:], L2[:], AX.X, ALU.max), reads=R, writes=R)
                    S.op("dve", lambda e, L=L, m2=m2, sel=sel: e.tensor_scalar(sel[:], L[:], m2[:, 0:1], None, ALU.is_ge), reads=R, writes=R)
                    S.op("dve", lambda e, m1=m1, nm1=nm1: e.tensor_scalar(nm1[:], m1[:], -1.0, None, ALU.mult), reads=R, writes=R)
                    S.op("act", lambda e, L=L, ex=ex, nm1=nm1: e.activation(ex[:], L[:], AF.Exp, bias=nm1[:, 0:1], scale=1.0), reads=R, writes=R)
                    S.op("dve", lambda e, ex=ex, sel=sel: e.tensor_tensor(ex[:], ex[:], sel[:], ALU.mult), reads=R, writes=R)
                    S.op("dve", lambda e, ex=ex, den=den: e.tensor_reduce(den[:], ex[:], AX.X, ALU.add), reads=R, writes=R)
                    S.op("dve", lambda e, den=den: e.reciprocal(den[:], den[:]), reads=R, writes=R)
                    S.op("dve", lambda e, ex=ex, den=den, t=t: e.tensor_scalar(gates[:, t, :], ex[:], den[:, 0:1], None, ALU.mult), reads=R, writes=[r_gates[t]])
            NTB2 = TSB // 256
            gi = 0
            for ex_ in range(NE):
                if moe:
                    Wg, Wu, Wd = Din["moe_w_gate"][j, ex_], Din["moe_w_up"][j, ex_], Din["moe_w_down"][j, ex_]
                else:
                    Wg, Wu, Wd = Din["ffn_w_gate"][j], Din["ffn_w_up"][j], Din["ffn_w_down"][j]
                for cg in range(NCG):
                    s = gi % 2; gi += 1
                    c0 = cg * GC * 128
                    S.dma("pool", wg[s][:], Wg[:, c0:c0 + GC * 128].rearrange("(k p) n -> p k n", p=128), writes=[r_wg[s]], sem=f"wg{s}")
                    S.dma("pool", wu[s][:], Wu[:, c0:c0 + GC * 128].rearrange("(k p) n -> p k n", p=128), writes=[r_wu[s]], sem=f"wu{s}")
                    S.dma("pool", wd[s][:], Wd[c0:c0 + GC * 128, :].rearrange("(c p) n -> p c n", p=128), writes=[r_wd[s]], sem=f"wd{s}")

                    def stageA(tb, s=s):
                        ab = tb % 2
                        for c in range(GC):
                            q = (tb * GC + c) % 2
                            for k in range(8):
                                S.op("pe", lambda e, q=q, c=c, k=k, tb=tb, s=s: e.matmul(pg[q][:, 0:256], wg[s][:, k, c * 128:(c + 1) * 128], zT[:, k, tb * 256:(tb + 1) * 256], start=(k == 0), stop=(k == 7)),
                                     reads=[r_wg[s], r_zT[tb]], writes=[r_pg[q]])
                            for k in range(8):
                                S.op("pe", lambda e, q=q, c=c, k=k, tb=tb, s=s: e.matmul(pu[q][:, 0:256], wu[s][:, k, c * 128:(c + 1) * 128], zT[:, k, tb * 256:(tb + 1) * 256], start=(k == 0), stop=(k == 7)),
                                     reads=[r_wu[s], r_zT[tb]], writes=[r_pu[q]])
                            g3 = (tb * GC + c) % 3
                            S.op("act", lambda e, q=q, g3=g3: e.activation(sg[g3][:, :], pg[q][:, 0:256], AF.Silu), reads=[r_pg[q]], writes=[r_sg[g3]])
                            S.op("dve", lambda e, q=q, g3=g3, ab=ab, c=c: e.tensor_tensor(aT[ab][:, c, :], pu[q][:, 0:256], sg[g3][:, :], ALU.mult),
                                 reads=[r_pu[q], r_sg[g3]], writes=[r_aT[ab]])

                    def stageB(tb, s=s, ex_=ex_):
                        ab = tb % 2
                        for tt in range(2):
                            t = tb * 2 + tt
                            for half in range(2):
                                for c in range(GC):
                                    S.op("pe", lambda e, tt=tt, half=half, c=c, ab=ab, s=s: e.matmul(pd[tt][half][:, :], aT[ab][:, c, tt * 128:(tt + 1) * 128], wd[s][:, c, half * 512:(half + 1) * 512],
                                                                                                    start=(c == 0), stop=(c == GC - 1)),
                                         reads=[r_aT[ab], r_wd[s]], writes=[r_pd[tt][half]])
                                dst = acc[:, t, half * 512:(half + 1) * 512]
                                if moe:
                                    S.op("dve", lambda e, tt=tt, half=half, dst=dst, t=t, ex_=ex_: e.scalar_tensor_tensor(dst, pd[tt][half][:, :], gates[:, t, ex_:ex_ + 1], dst, ALU.mult, ALU.add),
                                         reads=[r_pd[tt][half], r_gates[t], r_acc[t]], writes=[r_acc[t]])
                                else:
                                    S.op("dve", lambda e, tt=tt, half=half, dst=dst: e.tensor_tensor(dst, pd[tt][half][:, :], dst, ALU.add),
                                         reads=[r_pd[tt][half], r_acc[t]], writes=[r_acc[t]])

                    stageA(0)
                    for tb in range(NTB2):
                        if tb + 1 < NTB2:
                            stageA(tb + 1)
                        stageB(tb)
            for t in range(NTS):
                tg = sbk * NTS + t
                S.dma("sp", hout[tg * 128:(tg + 1) * 128, :], acc[:, t, :], reads=[r_acc[t]], writes=[r_h[tg]], sem="fst")
            P.end()

    hin = Din["hin"]
    for pi, ph in enumerate(plan):
        last = (pi == len(plan) - 1)
        hout = out if last else hbuf
        if ph[0] == "mix":
            mixer_phase(ph[1], hin)
        elif ph[0] == "out":
            outproj_phase(ph[1], hin, hout)
            hin = hout
        else:
            ffn_phase(ph[1], hin, hout, ph[2])
            hin = hout
    G.stack.close()
    S.close()
    return nc, {k: v[1] for k, v in declared.items()}


def make_consts():
    c = np.zeros((128, 512), np.float32)
    c[:, 0:128] = np.eye(128, dtype=np.float32)
    c[:, 128:256] = np.triu(np.ones((128, 128), np.float32))
    c[0:64, 256:320] = 1.0
    c[64:128, 320:384] = 1.0
    c[64, 384:512] = 1.0
    return c


_WEIGHT_KEYS = ["norm_mix", "norm_mem", "norm_ffn", "w_mem_kv", "g_mq", "g_mk", "fox_w_in", "fox_b_f", "fox_g_q", "fox_g_k",
                "fox_w_out", "conv_w_in", "conv_b_in", "conv_dw", "conv_dw_b", "conv_ln_g", "conv_ln_b", "conv_w_out",
                "ffn_w_gate", "ffn_w_up", "ffn_w_down", "moe_router", "moe_w_gate", "moe_w_up", "moe_w_down"]


def _full_plan():
    plan = []
    for i in range(4):
        plan += [("mix", i), ("out", i), ("ffn", i, (0, 1))]
    return plan


LAUNCHES = [_full_plan()]


def _launch(plan, inputs, h):
    nc, declared = build_program(plan)
    shared = {}
    for name, info in declared.items():
        if name in ("hin", "mem"):
            continue
        if name == "cst":
            shared[name] = make_consts()
            continue
        base, idx, row = info
        a = np.asarray(inputs[base][idx], dtype=np.float32)
        shared[name] = np.ascontiguousarray(a[None] if row else a)
    mem = np.asarray(inputs["mem"], dtype=np.float32)
    in_maps = []
    for b in range(8):
        m = dict(shared)
        m["hin"] = np.ascontiguousarray(h[b])
        if "mem" in declared:
            m["mem"] = np.ascontiguousarray(mem[b])
        in_maps.append(m)
    res = run_bass_kernel_spmd(nc, in_maps, core_ids=list(range(8)))
    return np.stack([np.asarray(r["out"], dtype=np.float32) for r in res.results], axis=0)


def kernel(**inputs):
    h = np.asarray(inputs["x"], dtype=np.float32)
    for plan in LAUNCHES:
        h = _launch(plan, inputs, h)
    return h
```

```python
from contextlib import ExitStack
import numpy as np
import concourse.bass as bass
import concourse.mybir as mybir
from concourse.bass_utils import run_bass_kernel_spmd

F32 = mybir.dt.float32
BF16 = mybir.dt.bfloat16
AF = mybir.ActivationFunctionType
ALU = mybir.AluOpType
AX = mybir.AxisListType

ENGS = ["pe", "act", "dve", "pool", "sp"]
BLOCKNAME = {"pe": "tensor", "act": "scalar", "dve": "vector", "pool": "gpsimd", "sp": "sync"}

S_TOK = 4096
D = 1024
NT = S_TOK // 128
NTB = S_TOK // 512
EPS = 1e-6
DFF = 2816
EFF = 3584
NEXP = 8


class Reg:
    __slots__ = ("w", "rs")

    def __init__(self):
        self.w = None
        self.rs = {}


def regs(n):
    return [Reg() for _ in range(n)]


class Sched:
    def __init__(self, nc):
        self.nc = nc
        self.ops = {e: [] for e in ENGS}
        self.done = {e: 0 for e in ENGS}
        self.cnt = {e: 0 for e in ENGS}
        self.waited = {e: {} for e in ENGS}
        self.dma_cnt = []
        self.dma_names = {}
        self.pstack = ExitStack()
        self.esem = {e: self.pstack.enter_context(nc.semaphore(f"es_{e}")) for e in ENGS}
        self.dsems = []
        self.n = 0

    def dsem(self, name):
        if name not in self.dma_names:
            self.dma_names[name] = len(self.dma_cnt)
            self.dma_cnt.append(0)
            self.dsems.append(self.pstack.enter_context(self.nc.semaphore(f"ds_{len(self.dsems)}")))
        return self.dma_names[name]

    def _need(self, eng, ticket, waits, raw):
        if ticket is None:
            return
        kind, key, val = ticket
        if kind == "E" and key == eng and (eng == "pe" or not raw):
            return
        k = (kind, key)
        if self.waited[eng].get(k, -1) >= val:
            return
        self.waited[eng][k] = val
        waits.append(ticket)
        if kind == "E":
            o = self.ops[key][val]
            assert not o.get("emitted") or o["sig"], "dependency on already-emitted unsignalled op"
            o["sig"] = True

    def _deps(self, eng, reads, writes):
        waits = []
        for r in reads:
            self._need(eng, r.w, waits, True)
        for r in writes:
            self._need(eng, r.w, waits, False)
            for (kind, key), val in r.rs.items():
                self._need(eng, (kind, key, val), waits, False)
        return waits

    def _commit(self, t, reads, writes):
        k = (t[0], t[1])
        for r in reads:
            if r.rs.get(k, -1) < t[2]:
                r.rs[k] = t[2]
        for r in writes:
            r.w = t
            r.rs = {}

    def op(self, eng, fn, reads=(), writes=()):
        waits = self._deps(eng, reads, writes)
        idx = len(self.ops[eng])
        self.ops[eng].append(dict(fn=fn, waits=waits, sig=False, dma=None))
        t = ("E", eng, idx)
        self._commit(t, reads, writes)
        return t

    def dma(self, q, out_ap, in_ap, reads=(), writes=(), sem="d"):
        waits = self._deps(q, reads, writes)
        sid = self.dsem(sem)
        if self.dma_cnt[sid] > 0:
            self._need(q, ("D", sid, self.dma_cnt[sid]), waits, True)
        self.dma_cnt[sid] += 16
        t = ("D", sid, self.dma_cnt[sid])
        self.ops[q].append(dict(fn=lambda e: e.dma_start(out=out_ap, in_=in_ap), waits=waits, sig=False, dma=sid))
        self._commit(t, reads, writes)
        return t

    def wait_only(self, eng, tickets):
        waits = []
        for t in tickets:
            self._need(eng, t, waits, True)
        self.ops[eng].append(dict(fn=None, waits=waits, sig=False, dma=None))

    def barrier(self):
        tickets = []
        for e in ENGS:
            for idx in range(len(self.ops[e]) - 1, self.done[e] - 1, -1):
                o = self.ops[e][idx]
                if o["fn"] is not None and o["dma"] is None:
                    tickets.append(("E", e, idx))
                    break
        for sid, c in enumerate(self.dma_cnt):
            if c > 0:
                tickets.append(("D", sid, c))
        for e in ENGS:
            self.wait_only(e, tickets)

    def flush(self):
        self.barrier()
        nc = self.nc
        ops = self.ops
        for e in ENGS:
            c = self.cnt[e]
            for o in ops[e][self.done[e]:]:
                if o["sig"]:
                    c += 1
                o["cnt"] = c
                o["emitted"] = True
            self.cnt[e] = c
        esem, dsems = self.esem, self.dsems

        def body(eng, e):
            for o in ops[e][self.done[e]:]:
                for (kind, key, val) in o["waits"]:
                    if kind == "E":
                        eng.wait_ge(esem[key], ops[key][val]["cnt"])
                    else:
                        eng.wait_ge(dsems[key], val)
                if o["fn"] is None:
                    continue
                ins = o["fn"](eng)
                if o["dma"] is not None:
                    ins.then_inc(dsems[o["dma"]], 16)
                elif o["sig"]:
                    ins.then_inc(esem[e], 1)

        with nc.Block() as block:
            for e in ENGS:
                if len(ops[e]) > self.done[e]:
                    getattr(block, BLOCKNAME[e])(lambda eng, e=e: body(eng, e))
        for e in ENGS:
            for o in ops[e][self.done[e]:]:
                o["fn"] = None if o["fn"] is None else True
            self.done[e] = len(ops[e])

    def close(self):
        self.pstack.close()


class Phase:
    def __init__(self, S):
        self.S = S
        self.stack = ExitStack()

    def sb(self, shape, dtype):
        self.S.n += 1
        return self.stack.enter_context(self.S.nc.sbuf_tensor(f"sb{self.S.n}", list(shape), dtype))

    def ps(self, shape=(128, 512), dtype=F32):
        self.S.n += 1
        return self.stack.enter_context(self.S.nc.psum_tensor(f"ps{self.S.n}", list(shape), dtype))

    def end(self):
        self.S.flush()
        self.stack.close()


def build_program(plan):
    nc = bass.Bass("TRN2", target_bir_lowering=False)
    FULL = dict(norm_mix=[4, D], norm_mem=[4, D], norm_ffn=[4, D], w_mem_kv=[4, D, 512], g_mq=[4, 64], g_mk=[4, 64],
                fox_w_in=[2, D, 2572], fox_b_f=[2, 12], fox_g_q=[2, 64], fox_g_k=[2, 64], fox_w_out=[2, D, D],
                conv_w_in=[2, D, 1792], conv_b_in=[2, 1536], conv_dw=[2, 31, 768], conv_dw_b=[2, 768], conv_ln_g=[2, 768],
                conv_ln_b=[2, 768], conv_w_out=[2, D, D], ffn_w_gate=[2, D, DFF], ffn_w_up=[2, D, DFF], ffn_w_down=[2, DFF, D],
                moe_router=[2, D, 8], moe_w_gate=[2, 8, D, EFF], moe_w_up=[2, 8, D, EFF], moe_w_down=[2, 8, EFF, D])
    declared = {}

    class LV:
        def __init__(self, name):
            self.name = name

        def _ap(self, idx, shape, row):
            nm = f"{self.name}_{idx}"
            if nm not in declared:
                declared[nm] = (nc.dram_tensor(nm, list(shape), F32, kind="ExternalInput").ap(), (self.name, idx, row))
            return declared[nm][0]

        def __getitem__(self, key):
            full = FULL[self.name]
            if isinstance(key, int):
                return self._ap(key, full[1:], False)
            if isinstance(key[0], slice):
                return self._ap(key[0].start, [1] + full[1:], True)
            return self._ap(key[0], full[1:], False)[key[1]]

    class LazyDin(dict):
        def __missing__(self, name):
            if name in FULL:
                v = LV(name)
            else:
                shape = dict(hin=[S_TOK, D], mem=[256, D], cst=[128, 512])[name]
                v = nc.dram_tensor(name, shape, F32, kind="ExternalInput").ap()
                declared[name] = (v, None)
            self[name] = v
            return v

    Din = LazyDin()
    out = nc.dram_tensor("out", [S_TOK, D], F32, kind="ExternalOutput").ap()
    hbuf = nc.dram_tensor("hbuf", [S_TOK, D], F32).ap()
    mixbuf = nc.dram_tensor("mixbuf", [8, 128, S_TOK], BF16).ap()
    r_h = regs(NT)
    r_mix = [regs(NTB) for _ in range(8)]

    S = Sched(nc)

    G = Phase(S)
    cst32 = G.sb([128, 512], F32); r_cst = Reg()
    cstb = G.sb([128, 512], BF16)
    epsT = G.sb([128, 1], F32); oneT = G.sb([128, 1], F32); zeroT = G.sb([128, 1], F32)
    onesF = G.sb([128, 128], F32); onesB = G.sb([128, 128], BF16)
    S.dma("sp", cst32[:], Din["cst"][:, :], writes=[r_cst], sem="cst")
    S.op("dve", lambda e: e.tensor_copy(cstb[:], cst32[:]), reads=[r_cst], writes=[r_cst])
    S.op("dve", lambda e: e.memset(epsT[:], EPS), writes=[r_cst])
    S.op("dve", lambda e: e.memset(oneT[:], 1.0), writes=[r_cst])
    S.op("dve", lambda e: e.memset(zeroT[:], 0.0), writes=[r_cst])
    S.op("dve", lambda e: e.memset(onesF[:], 1.0), writes=[r_cst])
    S.op("dve", lambda e: e.memset(onesB[:], 1.0), writes=[r_cst])
    identF = cst32[:, 0:128]; triF = cst32[:, 128:256]; selF = cst32[:, 384:512]
    identB = cstb[:, 0:128]; maskB = cstb[:, 128:256]; blkB = cstb[:, 256:384]
    S.flush()

    def rms_to_T(P, src_ap_fn, ntiles, gain_row_ap, dstT, r_dst_fn, pT, r_pT, tag):
        if not hasattr(P, "rms_tmp"):
            P.rms_tmp = (P.sb([128, D], F32), Reg(), [P.sb([128, D], F32) for _ in range(2)], regs(2),
                         P.sb([128, D], BF16), Reg(), [P.sb([128, 1], F32) for _ in range(2)], regs(2),
                         [P.sb([128, 1], F32) for _ in range(2)], regs(2), [P.sb([128, D], BF16) for _ in range(2)], regs(2))
        gbc, r_g, hl, r_hl, junk, r_junk, ss, r_ss, sd, r_sd, ub, r_ub = P.rms_tmp
        S.dma("sp", gbc[:], gain_row_ap.partition_broadcast(128), writes=[r_g], sem="rmsg")
        for t in range(ntiles):
            b = t % 2
            src, rsrc = src_ap_fn(t)
            S.dma("sp", hl[b][:], src, reads=rsrc, writes=[r_hl[b]], sem=f"{tag}h{b}")
            S.op("act", lambda e, b=b: e.activation(junk[:], hl[b][:], AF.Square, accum_out=ss[b][:, 0:1]),
                 reads=[r_hl[b]], writes=[r_junk, r_ss[b]])
            S.op("act", lambda e, b=b: e.activation(sd[b][:], ss[b][:], AF.Sqrt, bias=epsT[:, 0:1], scale=1.0 / D),
                 reads=[r_ss[b]], writes=[r_sd[b]])
            S.op("dve", lambda e, b=b: e.reciprocal(sd[b][:], sd[b][:]), reads=[r_sd[b]], writes=[r_sd[b]])
            S.op("dve", lambda e, b=b: e.scalar_tensor_tensor(ub[b][:], hl[b][:], sd[b][:, 0:1], gbc[:], ALU.mult, ALU.mult),
                 reads=[r_hl[b], r_sd[b], r_g], writes=[r_ub[b]])
            pt = pT[b]
            for k in range(8):
                S.op("pe", lambda e, b=b, k=k, pt=pt: e.transpose(pt[:, k * 128:(k + 1) * 128], ub[b][:, k * 128:(k + 1) * 128], identB),
                     reads=[r_ub[b], r_cst], writes=[r_pT[b]])
            eng = "act" if t % 2 == 0 else "dve"
            dst = dstT[:, :, t * 128:(t + 1) * 128]
            srcv = pt[:, :].rearrange("p (k n) -> p k n", k=8)
            if eng == "act":
                S.op("act", lambda e, dst=dst, srcv=srcv: e.copy(dst, srcv), reads=[r_pT[b]], writes=[r_dst_fn(t)])
            else:
                S.op("dve", lambda e, dst=dst, srcv=srcv: e.tensor_copy(dst, srcv), reads=[r_pT[b]], writes=[r_dst_fn(t)])

    def fm_norm(P, tmp, psrc, r_psrc, gcol, r_gcol, dst, r_dst, N, pS, r_pS, idx):
        sqb, r_sq, rs, r_rs = tmp
        b = idx % 2
        S.op("act", lambda e: e.activation(sqb[b][:, :N], psrc, AF.Square), reads=[r_psrc], writes=[r_sq[b]])
        S.op("pe", lambda e: e.matmul(pS[:, :N], blkB, sqb[b][:, :N], start=True, stop=True), reads=[r_sq[b], r_cst], writes=[r_pS])
        S.op("act", lambda e: e.activation(rs[b][:, :N], pS[:, :N], AF.Sqrt, bias=epsT[:, 0:1], scale=1.0 / 64),
             reads=[r_pS], writes=[r_rs[b]])
        S.op("dve", lambda e: e.reciprocal(rs[b][:, :N], rs[b][:, :N]), reads=[r_rs[b]], writes=[r_rs[b]])
        S.op("dve", lambda e: e.scalar_tensor_tensor(dst, psrc, gcol, rs[b][:, :N], ALU.mult, ALU.mult),
             reads=[r_psrc, r_rs[b], r_gcol], writes=[r_dst])

    def fm_tmp(P):
        return ([P.sb([128, 512], BF16) for _ in range(2)], regs(2), [P.sb([128, 512], F32) for _ in range(2)], regs(2))

    def load_gvec(P, rows, tag):
        n = len(rows)
        gv = P.sb([n, 128], F32); r_gv = Reg()
        for i, r in enumerate(rows):
            for hf in range(2):
                S.dma("sp", gv[i:i + 1, hf * 64:(hf + 1) * 64], r, writes=[r_gv], sem=tag)
        return gv, r_gv

    def mixer_phase(i, hin):
        j = i // 2
        fox = (i % 2 == 0)
        PO = Phase(S)
        if not fox:
            vt = PO.sb([36, 768], F32); r_vt = Reg()
            vT = PO.sb([128, 6, 36], F32); r_vT = Reg()
            ugT = PO.sb([128, 6, S_TOK + 32], BF16); r_ug = [regs(NTB) for _ in range(6)]
        P = Phase(S)
        uT = P.sb([128, 8, S_TOK], BF16); r_uT = regs(NTB)
        pA = [P.ps() for _ in range(2)]; r_pA = regs(2)
        pS = P.ps(); r_pS = Reg()
        pST = [P.ps() for _ in range(2)]; r_pST = regs(2)
        pO = [P.ps() for _ in range(2)]; r_pO = regs(2)
        pX = P.ps(); r_pX = Reg()
        pXb = pX[:, :].bitcast(BF16)
        pTb = [pXb, pO[0][:, :].bitcast(BF16)]
        rms_to_T(P, lambda t: (hin[t * 128:(t + 1) * 128, :], [r_h[t]]), NT, Din["norm_mix"][i:i + 1, :],
                 uT, lambda t: r_uT[t // 4], pTb, [r_pX, r_pO[0]], f"u{i}")
        tmp = fm_tmp(P)
        rows = ([Din["fox_g_q"][j:j + 1, :], Din["fox_g_k"][j:j + 1, :]] if fox else []) + \
               [Din["g_mq"][i:i + 1, :], Din["g_mk"][i:i + 1, :]]
        gv, r_gv = load_gvec(P, rows, f"gv{i}")
        gT = P.sb([128, 4], F32); r_gT = Reg()
        ng = len(rows)
        S.op("pe", lambda e: e.transpose(pS[:, 0:ng], gv[0:ng, :], identF[0:ng, 0:ng]), reads=[r_gv, r_cst], writes=[r_pS])
        S.op("dve", lambda e: e.tensor_copy(gT[:, 0:ng], pS[:, 0:ng]), reads=[r_pS], writes=[r_gT])
        c_gq, c_gk = (0, 1) if fox else (None, None)
        c_gmq, c_gmk = (2, 3) if fox else (0, 1)

        memT = P.sb([128, 8, 256], BF16); r_memT = Reg()
        rms_to_T(P, lambda t: (Din["mem"][t * 128:(t + 1) * 128, :], []), 2, Din["norm_mem"][i:i + 1, :],
                 memT, lambda t: r_memT, pTb, [r_pX, r_pO[0]], f"m{i}")
        wkv = P.sb([128, 8, 512], BF16); r_wkv = Reg()
        S.dma("pool", wkv[:], Din["w_mem_kv"][i].rearrange("(k p) n -> p k n", p=128), writes=[r_wkv], sem=f"wkv")
        kmT = P.sb([128, 2, 256], BF16); r_kmT = Reg()
        V1m = P.sb([128, 2, 4, 65], BF16); r_V1m = Reg()
        S.op("dve", lambda e: e.memset(V1m[:], 1.0), writes=[r_V1m])
        cnt = [0]

        def nxtA():
            cnt[0] += 1
            return pA[cnt[0] % 2], r_pA[cnt[0] % 2], cnt[0]

        for pr in range(2):
            p, rp, idx = nxtA()
            for k in range(8):
                S.op("pe", lambda e, p=p, k=k, pr=pr: e.matmul(p[:, 0:256], wkv[:, k, pr * 128:(pr + 1) * 128], memT[:, k, :], start=(k == 0), stop=(k == 7)),
                     reads=[r_wkv, r_memT], writes=[rp])
            fm_norm(P, tmp, p[:, 0:256], rp, gT[:, c_gmk:c_gmk + 1], r_gT, kmT[:, pr, :], r_kmT, 256, pS, r_pS, idx)
        for mt in range(2):
            p, rp, idx = nxtA()
            for k in range(8):
                S.op("pe", lambda e, p=p, k=k, mt=mt: e.matmul(p[:, 0:256], memT[:, k, mt * 128:(mt + 1) * 128], wkv[:, k, 256:512], start=(k == 0), stop=(k == 7)),
                     reads=[r_wkv, r_memT], writes=[rp])
            S.op("act", lambda e, p=p, mt=mt: e.copy(V1m[:, mt, :, 0:64], p[:, 0:256].rearrange("p (h d) -> p h d", h=4)),
                 reads=[rp], writes=[r_V1m])

        qT = P.sb([128, S_TOK], BF16); r_qT = regs(NTB)
        Pt = [P.sb([128, 512], BF16) for _ in range(3)]; r_Pt = regs(3)
        Osb = [P.sb([128, 512], F32) for _ in range(2)]; r_Osb = regs(2)
        rden = [P.sb([64, 512], F32) for _ in range(2)]; r_rden = regs(2)
        mixo = [P.sb([128, 512], BF16) for _ in range(2)]; r_mixo = regs(2)
        st = dict(p=0, o=0, m=0)

        def in_proj_fm(w, r_w, dstT, r_dstT, gcol):
            for tb in range(NTB):
                p, rp, idx = nxtA()
                for k in range(8):
                    S.op("pe", lambda e, p=p, k=k, tb=tb: e.matmul(p[:, :], w[:, k, :], uT[:, k, tb * 512:(tb + 1) * 512], start=(k == 0), stop=(k == 7)),
                         reads=[r_w, r_uT[tb]], writes=[rp])
                fm_norm(P, tmp, p[:, :], rp, gcol, r_gT, dstT[:, tb * 512:(tb + 1) * 512], r_dstT[tb], 512, pS, r_pS, idx)

        def finish_head(po, r_po, hp, qb, chunk, last):
            o = st["o"] % 2; st["o"] += 1
            m = st["m"] % 2
            S.op("dve", lambda e: e.tensor_copy(Osb[o][0:65, :], po[0:65, :]), reads=[r_po], writes=[r_Osb[o]])
            S.op("pe", lambda e: e.matmul(pS[0:64, :], selF[0:65, 0:64], Osb[o][0:65, :], start=True, stop=True),
                 reads=[r_Osb[o], r_cst], writes=[r_pS])
            S.op("dve", lambda e: e.reciprocal(rden[o][:, :], pS[0:64, :]), reads=[r_pS], writes=[r_rden[o]])
            S.op("dve", lambda e: e.tensor_tensor(mixo[m][hp * 64:(hp + 1) * 64, :], Osb[o][0:64, :], rden[o][:, :], ALU.mult),
                 reads=[r_Osb[o], r_rden[o]], writes=[r_mixo[m]])
            if last:
                S.dma("sp", mixbuf[chunk, :, qb * 512:(qb + 1) * 512], mixo[m][:, :], reads=[r_mixo[m]], writes=[r_mix[chunk][qb]], sem=f"mixo{m}")
                st["m"] += 1

        def mem_attention(mqT, r_mqT, pr):
            tasks = [(qb, hp, mt) for qb in range(NTB) for hp in range(2) for mt in range(2)]
            slots = {}

            def emit_qk(ti):
                qb, hp, mt = tasks[ti]
                pi = st["p"]; st["p"] += 1
                slots[ti] = pi
                ps_, rps = pST[pi % 2], r_pST[pi % 2]
                S.op("pe", lambda e, ps_=ps_, mt=mt, hp=hp, qb=qb: e.matmul(ps_[:, :], kmT[hp * 64:(hp + 1) * 64, pr, mt * 128:(mt + 1) * 128],
                                                                          mqT[hp * 64:(hp + 1) * 64, qb * 512:(qb + 1) * 512], start=True, stop=True),
                     reads=[r_kmT, r_mqT[qb]], writes=[rps])

            emit_qk(0)
            cur = {}
            for ti, (qb, hp, mt) in enumerate(tasks):
                if ti + 1 < len(tasks):
                    emit_qk(ti + 1)
                if mt == 0:
                    cur["po"], cur["r_po"] = pO[st["o"] % 2], r_pO[st["o"] % 2]
                po, r_po = cur["po"], cur["r_po"]
                pslot = slots.pop(ti)
                ps_, rps = pST[pslot % 2], r_pST[pslot % 2]
                pi = pslot % 3
                S.op("act", lambda e, ps_=ps_, pi=pi: e.activation(Pt[pi][:, :], ps_[:, :], AF.Exp, bias=zeroT[:, 0:1], scale=0.125),
                     reads=[rps], writes=[r_Pt[pi]])
                S.op("pe", lambda e, po=po, pi=pi, mt=mt, hp=hp: e.matmul(po[0:65, :], V1m[:, mt, 2 * pr + hp, :], Pt[pi][:, :], start=(mt == 0), stop=(mt == 1)),
                     reads=[r_V1m, r_Pt[pi]], writes=[r_po])
                if mt == 1:
                    finish_head(po, r_po, hp, qb, 6 + pr, hp == 1)

        Win = Din["fox_w_in"][j] if fox else Din["conv_w_in"][j]
        wq = P.sb([128, 8, 128], BF16); r_wq = Reg()
        wk = P.sb([128, 8, 128], BF16); r_wk = Reg()
        wv = P.sb([128, 8, 128], BF16); r_wv = Reg()

        def load_w(dst, r_dst, col0, ncol, sem):
            S.dma("pool", dst[:, :, 0:ncol], Win[:, col0:col0 + ncol].rearrange("(k p) n -> p k n", p=128), writes=[r_dst], sem=sem)

        if fox:
            kT = P.sb([128, S_TOK], BF16); r_kT = regs(NTB)
            V1 = P.sb([128, NT, 2, 65], BF16); r_V1 = regs(NTB)
            S.op("dve", lambda e: e.memset(V1[:], 1.0), writes=r_V1)
            wf = P.sb([128, 8, 12], BF16); r_wf = Reg()
            load_w(wf, r_wf, 2304, 12, "wf")
            bfb = P.sb([128, 12], F32); r_bfb = Reg()
            S.dma("sp", bfb[:], Din["fox_b_f"][j:j + 1, :].partition_broadcast(128), writes=[r_bfb], sem="bfb")
            Ctok = P.sb([128, 12, NT], F32); r_C = Reg()
            carry = P.sb([128, 12, NT + 1], F32); r_carry = Reg()
            S.op("dve", lambda e: e.memset(carry[:], 0.0), writes=[r_carry])
            nl = [P.sb([128, 12], F32) for _ in range(2)]; r_nl = regs(2)
            tl = [P.sb([128, 12], F32) for _ in range(2)]; r_tl = regs(2)
            for t in range(NT):
                b = t % 2
                p, rp, idx = nxtA()
                for k in range(8):
                    S.op("pe", lambda e, p=p, k=k, t=t: e.matmul(p[:, 0:12], uT[:, k, t * 128:(t + 1) * 128], wf[:, k, :], start=(k == 0), stop=(k == 7)),
                         reads=[r_wf, r_uT[t // 4]], writes=[rp])
                S.op("dve", lambda e, p=p, b=b: e.tensor_tensor(tl[b][:], p[:, 0:12], bfb[:], ALU.add), reads=[rp, r_bfb], writes=[r_tl[b]])
                S.op("act", lambda e, b=b: e.activation(tl[b][:], tl[b][:], AF.Exp, bias=zeroT[:, 0:1], scale=-1.0), reads=[r_tl[b]], writes=[r_tl[b]])
                S.op("act", lambda e, b=b: e.activation(nl[b][:], tl[b][:], AF.Ln, bias=oneT[:, 0:1], scale=1.0), reads=[r_tl[b]], writes=[r_nl[b]])
                S.op("pe", lambda e, b=b: e.matmul(pS[:, 0:12], triF, nl[b][:], start=True, stop=True), reads=[r_nl[b], r_cst], writes=[r_pS])
                S.op("pe", lambda e, b=b: e.matmul(pS[:, 16:28], onesF[:, :], nl[b][:], start=True, stop=True), reads=[r_nl[b], r_cst], writes=[r_pS])
                S.op("dve", lambda e, t=t: e.tensor_tensor(Ctok[:, :, t], pS[:, 0:12], carry[:, :, t], ALU.add), reads=[r_pS, r_carry], writes=[r_C])
                S.op("dve", lambda e, t=t: e.tensor_tensor(carry[:, :, t + 1], pS[:, 16:28], carry[:, :, t], ALU.add), reads=[r_pS, r_carry], writes=[r_carry])
            Bq = [P.sb([128, 4], F32) for _ in range(2)]; r_Bq = regs(2)
            nb = [0]
            for pj in range(6):
                load_w(wq, r_wq, pj * 128, 128, "wq")
                load_w(wk, r_wk, 768 + pj * 128, 128, "wk")
                load_w(wv, r_wv, 1536 + pj * 128, 128, "wv")
                in_proj_fm(wq, r_wq, qT, r_qT, gT[:, c_gq:c_gq + 1])
                in_proj_fm(wk, r_wk, kT, r_kT, gT[:, c_gk:c_gk + 1])
                for t in range(NT):
                    p, rp, idx = nxtA()
                    for k in range(8):
                        S.op("pe", lambda e, p=p, k=k, t=t: e.matmul(p[:, 0:128], uT[:, k, t * 128:(t + 1) * 128], wv[:, k, :], start=(k == 0), stop=(k == 7)),
                             reads=[r_wv, r_uT[t // 4]], writes=[rp])
                    ev = "act" if t % 2 == 0 else "dve"
                    dst = V1[:, t, :, 0:64]
                    srcv = p[:, 0:128].rearrange("p (h d) -> p h d", h=2)
                    if ev == "act":
                        S.op("act", lambda e, dst=dst, srcv=srcv: e.copy(dst, srcv), reads=[rp], writes=[r_V1[t // 4]])
                    else:
                        S.op("dve", lambda e, dst=dst, srcv=srcv: e.tensor_copy(dst, srcv), reads=[rp], writes=[r_V1[t // 4]])
                tasks = [(qb, hp, kt, 4 * qb + 4) for qb in range(NTB) for hp in range(2) for kt in range(4 * qb + 4)]
                slots = {}

                def emit_qk(ti):
                    qb, hp, kt, nk = tasks[ti]
                    pi = st["p"]; st["p"] += 1
                    slots[ti] = pi
                    ps_, rps = pST[pi % 2], r_pST[pi % 2]
                    S.op("pe", lambda e, ps_=ps_, kt=kt, hp=hp, qb=qb: e.matmul(ps_[:, :], kT[hp * 64:(hp + 1) * 64, kt * 128:(kt + 1) * 128],
                                                                              qT[hp * 64:(hp + 1) * 64, qb * 512:(qb + 1) * 512], start=True, stop=True),
                         reads=[r_kT[kt // 4], r_qT[qb]], writes=[rps])

                emit_qk(0)
                cur = {}
                for ti, (qb, hp, kt, nk) in enumerate(tasks):
                    if ti + 1 < len(tasks):
                        emit_qk(ti + 1)
                    h = 2 * pj + hp
                    if kt == 0:
                        cur["po"], cur["r_po"] = pO[st["o"] % 2], r_pO[st["o"] % 2]
                    po, r_po = cur["po"], cur["r_po"]
                    pslot = slots.pop(ti)
                    ps_, rps = pST[pslot % 2], r_pST[pslot % 2]
                    pi = pslot % 3
                    bq = nb[0] % 2; nb[0] += 1
                    S.op("dve", lambda e, bq=bq, h=h, qb=qb, kt=kt: e.tensor_scalar(Bq[bq][:, :], carry[:, h, 4 * qb:4 * qb + 4], Ctok[:, h, kt:kt + 1], -1.0, ALU.subtract, ALU.mult),
                         reads=[r_carry, r_C], writes=[r_Bq[bq]])
                    r = kt - 4 * qb
                    for jj in range(4):
                        if r > jj:
                            continue
                        S.op("act", lambda e, ps_=ps_, pi=pi, jj=jj, bq=bq: e.activation(Pt[pi][:, jj * 128:(jj + 1) * 128], ps_[:, jj * 128:(jj + 1) * 128], AF.Exp,
                                                                                        bias=Bq[bq][:, jj:jj + 1], scale=0.125),
                             reads=[rps, r_Bq[bq]], writes=[r_Pt[pi]])
                    if r >= 0:
                        S.op("dve", lambda e, pi=pi, r=r: e.tensor_tensor(Pt[pi][:, r * 128:(r + 1) * 128], Pt[pi][:, r * 128:(r + 1) * 128], maskB, ALU.mult),
                             reads=[r_Pt[pi], r_cst], writes=[r_Pt[pi]])
                        if r > 0:
                            S.op("dve", lambda e, pi=pi, r=r: e.memset(Pt[pi][:, 0:r * 128], 0.0), writes=[r_Pt[pi]])
                    S.op("pe", lambda e, po=po, pi=pi, kt=kt, hp=hp, nk=nk: e.matmul(po[0:65, :], V1[:, kt, hp, :], Pt[pi][:, :], start=(kt == 0), stop=(kt == nk - 1)),
                         reads=[r_V1[kt // 4], r_Pt[pi]], writes=[r_po])
                    if kt == nk - 1:
                        finish_head(po, r_po, hp, qb, pj, hp == 1)
            for pr in range(2):
                load_w(wq, r_wq, 2316 + pr * 128, 128, "wq")
                in_proj_fm(wq, r_wq, qT, r_qT, gT[:, c_gmq:c_gmq + 1])
                mem_attention(qT, r_qT, pr)
        else:
            S.dma("sp", vt[0:31, :], Din["conv_dw"][j], writes=[r_vt], sem="vt")
            S.dma("sp", vt[31:32, :], Din["conv_dw_b"][j:j + 1, :], writes=[r_vt], sem="vt")
            S.dma("sp", vt[32:33, :], Din["conv_ln_g"][j:j + 1, :], writes=[r_vt], sem="vt")
            S.dma("sp", vt[33:34, :], Din["conv_ln_b"][j:j + 1, :], writes=[r_vt], sem="vt")
            S.dma("sp", vt[34:36, :], Din["conv_b_in"][j].rearrange("(a n) -> a n", a=2), writes=[r_vt], sem="vt")
            for c in range(6):
                S.op("pe", lambda e, c=c: e.transpose(pS[:, 0:36], vt[0:36, c * 128:(c + 1) * 128], identF[0:36, 0:36]), reads=[r_vt, r_cst], writes=[r_pS])
                S.op("dve", lambda e, c=c: e.tensor_copy(vT[:, c, :], pS[:, 0:36]), reads=[r_pS], writes=[r_vT])
            S.op("dve", lambda e: e.memset(ugT[:, :, 0:32], 0.0), writes=[r_ug[c][0] for c in range(6)])
            sgm = [P.sb([128, 512], F32) for _ in range(2)]; r_sgm = regs(2)
            OFF = 32
            for c in range(6):
                load_w(wq, r_wq, c * 128, 128, "wq")
                load_w(wk, r_wk, 768 + c * 128, 128, "wk")
                for tb in range(NTB):
                    pa, rpa, _ = nxtA()
                    pb, rpb, idx = nxtA()
                    for k in range(8):
                        S.op("pe", lambda e, pa=pa, k=k, tb=tb: e.matmul(pa[:, :], wq[:, k, :], uT[:, k, tb * 512:(tb + 1) * 512], start=(k == 0), stop=(k == 7)),
                             reads=[r_wq, r_uT[tb]], writes=[rpa])
                    for k in range(8):
                        S.op("pe", lambda e, pb=pb, k=k, tb=tb: e.matmul(pb[:, :], wk[:, k, :], uT[:, k, tb * 512:(tb + 1) * 512], start=(k == 0), stop=(k == 7)),
                             reads=[r_wk, r_uT[tb]], writes=[rpb])
                    b = idx % 2
                    S.op("act", lambda e, pb=pb, b=b, c=c: e.activation(sgm[b][:, :], pb[:, :], AF.Sigmoid, bias=vT[:, c, 35:36], scale=1.0),
                         reads=[rpb, r_vT], writes=[r_sgm[b]])
                    S.op("dve", lambda e, pa=pa, b=b, c=c, tb=tb: e.scalar_tensor_tensor(ugT[:, c, OFF + tb * 512:OFF + (tb + 1) * 512], pa[:, :], vT[:, c, 34:35], sgm[b][:, :], ALU.add, ALU.mult),
                         reads=[rpa, r_sgm[b], r_vT], writes=[r_ug[c][tb]])
            for pr in range(2):
                load_w(wv, r_wv, 1536 + pr * 128, 128, "wv")
                in_proj_fm(wv, r_wv, qT, r_qT, gT[:, c_gmq:c_gmq + 1])
                mem_attention(qT, r_qT, pr)
            P.end()
            P = Phase(S)
            xc = [P.sb([128, 6, 512], F32) for _ in range(2)]; r_xc = [regs(6) for _ in range(2)]
            sq = P.sb([128, 512], F32); r_sqx = Reg()
            mean = P.sb([128, 512], F32); r_mean = Reg()
            rstd = P.sb([128, 512], F32); r_rstd = Reg()
            t1 = [P.sb([128, 512], F32) for _ in range(2)]; r_t1 = regs(2)
            mixo = [P.sb([128, 512], BF16) for _ in range(2)]; r_mixo = regs(2)
            pM, r_pM = P.ps(), Reg()
            pV, r_pV = P.ps(), Reg()
            for tb in range(NTB):
                xb = tb % 2
                base = OFF + tb * 512 - 30
                for w in range(31):
                    for c in range(6):
                        acc = xc[xb][:, c, :]
                        rd = [r_ug[c][tb]] + ([r_ug[c][tb - 1]] if tb > 0 else []) + [r_vT]
                        if w == 0:
                            S.op("dve", lambda e, acc=acc, c=c, base=base: e.tensor_scalar(acc, ugT[:, c, base:base + 512], vT[:, c, 0:1], vT[:, c, 31:32], ALU.mult, ALU.add),
                                 reads=rd, writes=[r_xc[xb][c]])
                        else:
                            S.op("dve", lambda e, acc=acc, c=c, base=base, w=w: e.scalar_tensor_tensor(acc, ugT[:, c, base + w:base + w + 512], vT[:, c, w:w + 1], acc, ALU.mult, ALU.add),
                                 reads=rd + [r_xc[xb][c]], writes=[r_xc[xb][c]])
                for c in range(6):
                    S.op("pe", lambda e, c=c, xb=xb: e.matmul(pM[:, :], onesF[:, :], xc[xb][:, c, :], start=(c == 0), stop=(c == 5)),
                         reads=[r_xc[xb][c], r_cst], writes=[r_pM])
                for c in range(6):
                    S.op("act", lambda e, c=c, xb=xb: e.activation(sq[:, :], xc[xb][:, c, :], AF.Square), reads=[r_xc[xb][c]], writes=[r_sqx])
                    S.op("pe", lambda e, c=c: e.matmul(pV[:, :], onesF[:, :], sq[:, :], start=(c == 0), stop=(c == 5)),
                         reads=[r_sqx, r_cst], writes=[r_pV])
                S.op("dve", lambda e: e.tensor_scalar(mean[:, :], pM[:, :], 1.0 / 768, None, ALU.mult), reads=[r_pM], writes=[r_mean])
                S.op("dve", lambda e: e.tensor_tensor(rstd[:, :], mean[:, :], mean[:, :], ALU.mult), reads=[r_mean], writes=[r_rstd])
                S.op("dve", lambda e: e.scalar_tensor_tensor(rstd[:, :], pV[:, :], 1.0 / 768, rstd[:, :], ALU.mult, ALU.subtract), reads=[r_pV, r_rstd], writes=[r_rstd])
                S.op("act", lambda e: e.activation(rstd[:, :], rstd[:, :], AF.Sqrt, bias=epsT[:, 0:1], scale=1.0), reads=[r_rstd], writes=[r_rstd])
                S.op("dve", lambda e: e.reciprocal(rstd[:, :], rstd[:, :]), reads=[r_rstd], writes=[r_rstd])
                for c in range(6):
                    tt = c % 2
                    m = st["m"] % 2; st["m"] += 1
                    S.op("dve", lambda e, c=c, xb=xb, tt=tt: e.tensor_tensor(t1[tt][:, :], xc[xb][:, c, :], mean[:, :], ALU.subtract), reads=[r_xc[xb][c], r_mean], writes=[r_t1[tt]])
                    S.op("dve", lambda e, tt=tt: e.tensor_tensor(t1[tt][:, :], t1[tt][:, :], rstd[:, :], ALU.mult), reads=[r_t1[tt], r_rstd], writes=[r_t1[tt]])
                    S.op("act", lambda e, c=c, tt=tt, m=m: e.activation(mixo[m][:, :], t1[tt][:, :], AF.Silu, bias=vT[:, c, 33:34], scale=vT[:, c, 32:33]),
                         reads=[r_t1[tt], r_vT], writes=[r_mixo[m]])
                    S.dma("sp", mixbuf[c, :, tb * 512:(tb + 1) * 512], mixo[m][:, :], reads=[r_mixo[m]], writes=[r_mix[c][tb]], sem=f"mixo{m}")
        P.end()
        PO.stack.close()

    def outproj_phase(i, hin, hout):
        j = i // 2
        Wo = Din["fox_w_out"][j] if i % 2 == 0 else Din["conv_w_out"][j]
        P = Phase(S)
        wout = P.sb([128, 8, D], BF16); r_wo = Reg()
        S.dma("pool", wout[:], Wo.rearrange("(k p) n -> p k n", p=128), writes=[r_wo], sem="wout")
        mixl = [P.sb([128, 8, 512], BF16) for _ in range(2)]; r_ml = regs(2)
        hl = [P.sb([128, D], F32) for _ in range(3)]; r_hl = regs(3)
        pd = [[P.ps() for _ in range(2)] for _ in range(2)]; r_pd = [regs(2) for _ in range(2)]
        for tb in range(NTB):
            mb = tb % 2
            S.dma("sp", mixl[mb][:], mixbuf[:, :, tb * 512:(tb + 1) * 512].rearrange("c p n -> p c n"),
                  reads=[r_mix[c][tb] for c in range(8)], writes=[r_ml[mb]], sem=f"ml{mb}")
            for tt in range(4):
                t = tb * 4 + tt
                hb = t % 3
                pb = t % 2
                S.dma("sp", hl[hb][:], hin[t * 128:(t + 1) * 128, :], reads=[r_h[t]], writes=[r_hl[hb]], sem=f"oh{hb}")
                for half in range(2):
                    for c in range(8):
                        S.op("pe", lambda e, pb=pb, half=half, c=c, mb=mb, tt=tt: e.matmul(pd[pb][half][:, :], mixl[mb][:, c, tt * 128:(tt + 1) * 128], wout[:, c, half * 512:(half + 1) * 512],
                                                                                          start=(c == 0), stop=(c == 7)),
                             reads=[r_ml[mb], r_wo], writes=[r_pd[pb][half]])
                    S.op("dve", lambda e, pb=pb, half=half, hb=hb: e.tensor_tensor(hl[hb][:, half * 512:(half + 1) * 512], pd[pb][half][:, :], hl[hb][:, half * 512:(half + 1) * 512], ALU.add),
                         reads=[r_pd[pb][half], r_hl[hb]], writes=[r_hl[hb]])
                S.dma("sp", hout[t * 128:(t + 1) * 128, :], hl[hb][:], reads=[r_hl[hb]], writes=[r_h[t]], sem=f"os{hb}")
        P.end()

    def ffn_phase(i, hin, hout, sbks=(0, 1)):
        j = i // 2
        moe = (i % 2 == 1)
        NE = NEXP if moe else 1
        FF = EFF if moe else DFF
        GC = 4 if moe else 2
        NCG = FF // 128 // GC
        NSB = 2
        TSB = S_TOK // NSB
        NTS = TSB // 128
        for sbk in sbks:
            P = Phase(S)
            acc = P.sb([128, NTS, D], F32); r_acc = regs(NTS)
            zT = P.sb([128, 8, TSB], BF16); r_zT = regs(TSB // 256)
            gates = P.sb([128, NTS, 8], F32); r_gates = regs(NTS)
            gbc = P.sb([128, D], F32); r_g = Reg()
            S.dma("sp", gbc[:], Din["norm_ffn"][i:i + 1, :].partition_broadcast(128), writes=[r_g], sem="fg")
            wg = [P.sb([128, 8, GC * 128], BF16) for _ in range(2)]; r_wg = regs(2)
            wu = [P.sb([128, 8, GC * 128], BF16) for _ in range(2)]; r_wu = regs(2)
            wd = [P.sb([128, GC, D], BF16) for _ in range(2)]; r_wd = regs(2)
            aT = [P.sb([128, GC, 256], BF16) for _ in range(2)]; r_aT = regs(2)
            sg = [P.sb([128, 256], F32) for _ in range(3)]; r_sg = regs(3)
            pd = [[P.ps() for _ in range(2)] for _ in range(2)]; r_pd = [regs(2) for _ in range(2)]
            pg = [P.ps() for _ in range(2)]; r_pg = regs(2)
            pu = [P.ps() for _ in range(2)]; r_pu = regs(2)
            junk = P.sb([128, D], BF16); r_junk = Reg()
            ss = [P.sb([128, 1], F32) for _ in range(2)]; r_ss = regs(2)
            sd = [P.sb([128, 1], F32) for _ in range(2)]; r_sd = regs(2)
            z32 = [P.sb([128, D], F32) for _ in range(2)]; r_z32 = regs(2)
            if moe:
                wr = P.sb([128, 8, 8], F32); r_wr = Reg()
                wrA = P.sb([8, 1024], F32); r_wrA = Reg()
                S.dma("sp", wrA[:], Din["moe_router"][j].rearrange("(i r) n -> i (r n)", i=8), writes=[r_wrA], sem="wr")
                wrAv = wrA[0:8, :].rearrange("i (r n) -> i r n", n=8)
                for n in range(8):
                    S.op("pe", lambda e, n=n: e.transpose(pg[0][:, n * 8:(n + 1) * 8], wrAv[:, :, n], identF[0:8, 0:8]), reads=[r_wrA, r_cst], writes=[r_pg[0]])
                S.op("act", lambda e: e.copy(wr[:, :, :].rearrange("p k n -> p n k"), pg[0][:, 0:64].rearrange("p (n k) -> p n k", n=8)), reads=[r_pg[0]], writes=[r_wr])
                zT32 = [P.sb([128, 8, 128], F32) for _ in range(2)]; r_zT32 = regs(2)
                rt = {n: [P.sb([128, 8], F32) for _ in range(2)] for n in ("L", "eq", "L2", "sel", "ex")}
                rc = {n: [P.sb([128, 1], F32) for _ in range(2)] for n in ("m1", "m2", "nm1", "den")}
                r_rt = regs(2)
            for t in range(NTS):
                b = t % 2
                tg = sbk * NTS + t
                S.dma("sp", acc[:, t, :], hin[tg * 128:(tg + 1) * 128, :], reads=[r_h[tg]], writes=[r_acc[t]], sem=f"fh{b}")
                S.op("act", lambda e, b=b, t=t: e.activation(junk[:], acc[:, t, :], AF.Square, accum_out=ss[b][:, 0:1]),
                     reads=[r_acc[t]], writes=[r_junk, r_ss[b]])
                S.op("act", lambda e, b=b: e.activation(sd[b][:], ss[b][:], AF.Sqrt, bias=epsT[:, 0:1], scale=1.0 / D), reads=[r_ss[b]], writes=[r_sd[b]])
                S.op("dve", lambda e, b=b: e.reciprocal(sd[b][:], sd[b][:]), reads=[r_sd[b]], writes=[r_sd[b]])
                S.op("dve", lambda e, b=b, t=t: e.scalar_tensor_tensor(z32[b][:], acc[:, t, :], sd[b][:, 0:1], gbc[:], ALU.mult, ALU.mult),
                     reads=[r_acc[t], r_sd[b], r_g], writes=[r_z32[b]])
                for hf in range(2):
                    pt, rpt = pg[hf], r_pg[hf]
                    for kk in range(4):
                        k = hf * 4 + kk
                        S.op("pe", lambda e, pt=pt, kk=kk, k=k, b=b: e.transpose(pt[:, kk * 128:(kk + 1) * 128], z32[b][:, k * 128:(k + 1) * 128], identF),
                             reads=[r_z32[b], r_cst], writes=[rpt])
                    S.op("act", lambda e, pt=pt, hf=hf, t=t: e.copy(zT[:, hf * 4:(hf + 1) * 4, t * 128:(t + 1) * 128], pt[:, :].rearrange("p (k n) -> p k n", k=4)),
                         reads=[rpt], writes=[r_zT[t // 2]])
                    if moe:
                        S.op("act", lambda e, pt=pt, hf=hf, b=b: e.copy(zT32[b][:, hf * 4:(hf + 1) * 4, :], pt[:, :].rearrange("p (k n) -> p k n", k=4)),
                             reads=[rpt], writes=[r_zT32[b]])
                if moe:
                    pl, rpl = pu[b], r_pu[b]
                    for k in range(8):
                        S.op("pe", lambda e, pl=pl, k=k, b=b: e.matmul(pl[:, 0:8], zT32[b][:, k, :], wr[:, k, :], start=(k == 0), stop=(k == 7)),
                             reads=[r_zT32[b], r_wr], writes=[rpl])
                    L, eq, L2, sel, ex = (rt[n][b] for n in ("L", "eq", "L2", "sel", "ex"))
                    m1, m2, nm1, den = (rc[n][b] for n in ("m1", "m2", "nm1", "den"))
                    R = [r_rt[b]]
                    S.op("dve", lambda e, L=L, pl=pl: e.tensor_copy(L[:], pl[:, 0:8]), reads=[rpl], writes=R)
                    S.op("dve", lambda e, L=L, m1=m1: e.tensor_reduce(m1[:], L[:], AX.X, ALU.max), reads=R, writes=R)
                    S.op("dve", lambda e, L=L, m1=m1, eq=eq: e.tensor_scalar(eq[:], L[:], m1[:, 0:1], None, ALU.is_ge), reads=R, writes=R)
                    S.op("dve", lambda e, L=L, L2=L2, eq=eq: e.scalar_tensor_tensor(L2[:], eq[:], -1e30, L[:], ALU.mult, ALU.add), reads=R, writes=R)
                    S.op("dve", lambda e, L2=L2, m2=m2: e.tensor_reduce(m2[:], L2[:], AX.X, ALU.max), reads=R, writes=R)
                    S.op("dve", lambda e, L=L, m2=m2, sel=sel: e.tensor_scalar(sel[:], L[:], m2[:, 0:1], None, ALU.is_ge), reads=R, writes=R)
                    S.op("dve", lambda e, m1=m1, nm1=nm1: e.tensor_scalar(nm1[:], m1[:], -1.0, None, ALU.mult), reads=R, writes=R)
                    S.op("act", lambda e, L=L, ex=ex, nm1=nm1: e.activation(ex[:], L[:], AF.Exp, bias=nm1[:, 0:1], scale=1.0), reads=R, writes=R)
                    S.op("dve", lambda e, ex=ex, sel=sel: e.tensor_tensor(ex[:], ex[:], sel[:], ALU.mult), reads=R, writes=R)
                    S.op("dve", lambda e, ex=ex, den=den: e.tensor_reduce(den[:], ex[:], AX.X, ALU.add), reads=R, writes=R)
                    S.op("dve", lambda e, den=den: e.reciprocal(den[:], den[:]), reads=R, writes=R)
                    S.op("dve", lambda e, ex=ex, den=den, t=t: e.tensor_scalar(gates[:, t, :], ex[:], den[:, 0:1], None, ALU.mult), reads=R, writes=[r_gates[t]])
            NTB2 = TSB // 256
            gi = 0
            for ex_ in range(NE):
                if moe:
                    Wg, Wu, Wd = Din["moe_w_gate"][j, ex_], Din["moe_w_up"][j, ex_], Din["moe_w_down"][j, ex_]
                else:
                    Wg, Wu, Wd = Din["ffn_w_gate"][j], Din["ffn_w_up"][j], Din["ffn_w_down"][j]
                for cg in range(NCG):
                    s = gi % 2; gi += 1
                    c0 = cg * GC * 128
                    S.dma("pool", wg[s][:], Wg[:, c0:c0 + GC * 128].rearrange("(k p) n -> p k n", p=128), writes=[r_wg[s]], sem=f"wg{s}")
                    S.dma("pool", wu[s][:], Wu[:, c0:c0 + GC * 128].rearrange("(k p) n -> p k n", p=128), writes=[r_wu[s]], sem=f"wu{s}")
                    S.dma("pool", wd[s][:], Wd[c0:c0 + GC * 128, :].rearrange("(c p) n -> p c n", p=128), writes=[r_wd[s]], sem=f"wd{s}")

                    def stageA(tb, s=s):
                        ab = tb % 2
                        for c in range(GC):
                            q = (tb * GC + c) % 2
                            for k in range(8):
                                S.op("pe", lambda e, q=q, c=c, k=k, tb=tb, s=s: e.matmul(pg[q][:, 0:256], wg[s][:, k, c * 128:(c + 1) * 128], zT[:, k, tb * 256:(tb + 1) * 256], start=(k == 0), stop=(k == 7)),
                                     reads=[r_wg[s], r_zT[tb]], writes=[r_pg[q]])
                            for k in range(8):
                                S.op("pe", lambda e, q=q, c=c, k=k, tb=tb, s=s: e.matmul(pu[q][:, 0:256], wu[s][:, k, c * 128:(c + 1) * 128], zT[:, k, tb * 256:(tb + 1) * 256], start=(k == 0), stop=(k == 7)),
                                     reads=[r_wu[s], r_zT[tb]], writes=[r_pu[q]])
                            g3 = (tb * GC + c) % 3
                            S.op("act", lambda e, q=q, g3=g3: e.activation(sg[g3][:, :], pg[q][:, 0:256], AF.Silu), reads=[r_pg[q]], writes=[r_sg[g3]])
                            S.op("dve", lambda e, q=q, g3=g3, ab=ab, c=c: e.tensor_tensor(aT[ab][:, c, :], pu[q][:, 0:256], sg[g3][:, :], ALU.mult),
                                 reads=[r_pu[q], r_sg[g3]], writes=[r_aT[ab]])

                    def stageB(tb, s=s, ex_=ex_):
                        ab = tb % 2
                        for tt in range(2):
                            t = tb * 2 + tt
                            for half in range(2):
                                for c in range(GC):
                                    S.op("pe", lambda e, tt=tt, half=half, c=c, ab=ab, s=s: e.matmul(pd[tt][half][:, :], aT[ab][:, c, tt * 128:(tt + 1) * 128], wd[s][:, c, half * 512:(half + 1) * 512],
                                                                                                    start=(c == 0), stop=(c == GC - 1)),
                                         reads=[r_aT[ab], r_wd[s]], writes=[r_pd[tt][half]])
                                dst = acc[:, t, half * 512:(half + 1) * 512]
                                if moe:
                                    S.op("dve", lambda e, tt=tt, half=half, dst=dst, t=t, ex_=ex_: e.scalar_tensor_tensor(dst, pd[tt][half][:, :], gates[:, t, ex_:ex_ + 1], dst, ALU.mult, ALU.add),
                                         reads=[r_pd[tt][half], r_gates[t], r_acc[t]], writes=[r_acc[t]])
                                else:
                                    S.op("dve", lambda e, tt=tt, half=half, dst=dst: e.tensor_tensor(dst, pd[tt][half][:, :], dst, ALU.add),
                                         reads=[r_pd[tt][half], r_acc[t]], writes=[r_acc[t]])

                    stageA(0)
                    for tb in range(NTB2):
                        if tb + 1 < NTB2:
                            stageA(tb + 1)
                        stageB(tb)
            for t in range(NTS):
                tg = sbk * NTS + t
                S.dma("sp", hout[tg * 128:(tg + 1) * 128, :], acc[:, t, :], reads=[r_acc[t]], writes=[r_h[tg]], sem="fst")
            P.end()

    hin = Din["hin"]
    for pi, ph in enumerate(plan):
        last = (pi == len(plan) - 1)
        hout = out if last else hbuf
        if ph[0] == "mix":
            mixer_phase(ph[1], hin)
        elif ph[0] == "out":
            outproj_phase(ph[1], hin, hout)
            hin = hout
        else:
            ffn_phase(ph[1], hin, hout, ph[2])
            hin = hout
    G.stack.close()
    S.close()
    return nc, {k: v[1] for k, v in declared.items()}


def make_consts():
    c = np.zeros((128, 512), np.float32)
    c[:, 0:128] = np.eye(128, dtype=np.float32)
    c[:, 128:256] = np.triu(np.ones((128, 128), np.float32))
    c[0:64, 256:320] = 1.0
    c[64:128, 320:384] = 1.0
    c[64, 384:512] = 1.0
    return c


_WEIGHT_KEYS = ["norm_mix", "norm_mem", "norm_ffn", "w_mem_kv", "g_mq", "g_mk", "fox_w_in", "fox_b_f", "fox_g_q", "fox_g_k",
                "fox_w_out", "conv_w_in", "conv_b_in", "conv_dw", "conv_dw_b", "conv_ln_g", "conv_ln_b", "conv_w_out",
                "ffn_w_gate", "ffn_w_up", "ffn_w_down", "moe_router", "moe_w_gate", "moe_w_up", "moe_w_down"]


def _full_plan():
    plan = []
    for i in range(4):
        plan += [("mix", i), ("out", i), ("ffn", i, (0, 1))]
    return plan


LAUNCHES = [_full_plan()]


def _launch(plan, inputs, h):
    nc, declared = build_program(plan)
    shared = {}
    for name, info in declared.items():
        if name in ("hin", "mem"):
            continue
        if name == "cst":
            shared[name] = make_consts()
            continue
        base, idx, row = info
        a = np.asarray(inputs[base][idx], dtype=np.float32)
        shared[name] = np.ascontiguousarray(a[None] if row else a)
    mem = np.asarray(inputs["mem"], dtype=np.float32)
    in_maps = []
    for b in range(8):
        m = dict(shared)
        m["hin"] = np.ascontiguousarray(h[b])
        if "mem" in declared:
            m["mem"] = np.ascontiguousarray(mem[b])
        in_maps.append(m)
    res = run_bass_kernel_spmd(nc, in_maps, core_ids=list(range(8)))
    return np.stack([np.asarray(r["out"], dtype=np.float32) for r in res.results], axis=0)


def kernel(**inputs):
    h = np.asarray(inputs["x"], dtype=np.float32)
    for plan in LAUNCHES:
        h = _launch(plan, inputs, h)
    return h
```
